# Optimizing a Trainium2 kernel written in Bass

```python
import jax, jax.numpy as jnp
from jax import lax
import numpy as np

D_MODEL = 1024
BATCH = 16
SEQ = 4096
DEPTH = 2

CHUNK = 64
HEAD_DIM = 64
A_HEADS = 4
A_LEFT_CHUNKS = 8
A_MAX_REL = 128
B_HEADS = 4
C_HEADS = 4
IDX_HEADS = 4
IDX_DIM = 64
TOPK_MAX = 256
N_BRANCH = 3
MIX_W = 256
D_FF = 2816
CONV_W = 3
Q_BLOCK = 128
ROPE_THETA = 10000.0
EPS = 1e-6
NEG = -1e30

COL_SIZES = (MIX_W, MIX_W, MIX_W,
             MIX_W, MIX_W, MIX_W, B_HEADS,
             MIX_W, HEAD_DIM, HEAD_DIM,
             IDX_HEADS * IDX_DIM, IDX_DIM, IDX_HEADS,
             N_BRANCH * D_MODEL)
N_IN = sum(COL_SIZES)

kernel_name = "hybrid_streaming_encoder"


def _rmsnorm(x, g):
    xf = x.astype(jnp.float32)
    y = xf * lax.rsqrt(jnp.mean(xf * xf, axis=-1, keepdims=True) + EPS)
    return (y * g.astype(jnp.float32)).astype(x.dtype)


def _rope(x, cos, sin):
    xf = x.astype(jnp.float32)
    x1, x2 = jnp.split(xf, 2, axis=-1)
    c = cos[None, :, None, :]
    s = sin[None, :, None, :]
    return jnp.concatenate([x1 * c - x2 * s, x1 * s + x2 * c], axis=-1).astype(x.dtype)


def _blocks_to_seq(out):
    nb, b, blk, h, d = out.shape
    return out.transpose(1, 0, 2, 3, 4).reshape(b, nb * blk, h, d)


def _chunk_band_attention(q, k, v, rel_table):
    S = q.shape[1]
    dh = q.shape[-1]
    n_chunks = S // CHUNK
    pad = A_LEFT_CHUNKS * CHUNK
    band = pad + CHUNK
    kp = jnp.pad(k, ((0, 0), (pad, 0), (0, 0), (0, 0)))
    vp = jnp.pad(v, ((0, 0), (pad, 0), (0, 0), (0, 0)))
    qi = jnp.arange(CHUNK)
    kj = jnp.arange(band)
    rel = qi[:, None] + pad - kj[None, :]
    bias = rel_table[:, jnp.clip(rel, -A_MAX_REL, A_MAX_REL) + A_MAX_REL].astype(jnp.float32)
    scale = dh ** -0.5

    def one_chunk(n):
        start = n * CHUNK
        qc = lax.dynamic_slice_in_dim(q, start, CHUNK, axis=1)
        kc = lax.dynamic_slice_in_dim(kp, start, band, axis=1)
        vc = lax.dynamic_slice_in_dim(vp, start, band, axis=1)
        s = jnp.einsum('bqhd,bkhd->bhqk', qc, kc).astype(jnp.float32) * scale + bias
        valid = (start - pad + kj) >= 0
        s = jnp.where(valid[None, None, None, :], s, NEG)
        p = jax.nn.softmax(s, axis=-1).astype(v.dtype)
        return jnp.einsum('bhqk,bkhd->bqhd', p, vc)

    return _blocks_to_seq(lax.map(one_chunk, jnp.arange(n_chunks)))


def _forgetting_attention(q, k, v, log_f):
    S = q.shape[1]
    dh = q.shape[-1]
    cum = jnp.cumsum(log_f, axis=1).transpose(0, 2, 1)
    kpos = jnp.arange(S)
    scale = dh ** -0.5

    def one_block(i):
        start = i * Q_BLOCK
        qb = lax.dynamic_slice_in_dim(q, start, Q_BLOCK, axis=1)
        cq = lax.dynamic_slice_in_dim(cum, start, Q_BLOCK, axis=2)
        s = jnp.einsum('bqhd,bkhd->bhqk', qb, k).astype(jnp.float32) * scale
        s = s + cq[..., :, None] - cum[:, :, None, :]
        qpos = start + jnp.arange(Q_BLOCK)
        s = jnp.where((kpos[None, :] <= qpos[:, None])[None, None], s, NEG)
        p = jax.nn.softmax(s, axis=-1).astype(v.dtype)
        return jnp.einsum('bhqk,bkhd->bqhd', p, v)

    return _blocks_to_seq(lax.map(one_block, jnp.arange(S // Q_BLOCK)))


def _indexed_sparse_attention(q, k, v, q_idx, k_idx, w_idx):
    S = q.shape[1]
    dh = q.shape[-1]
    topk = min(TOPK_MAX, S // 4)
    chunk_k = jnp.arange(S) // CHUNK
    scale = dh ** -0.5

    def one_block(i):
        start = i * Q_BLOCK
        qb = lax.dynamic_slice_in_dim(q, start, Q_BLOCK, axis=1)
        qib = lax.dynamic_slice_in_dim(q_idx, start, Q_BLOCK, axis=1)
        wb = lax.dynamic_slice_in_dim(w_idx, start, Q_BLOCK, axis=1).astype(jnp.float32)
        logits = jnp.einsum('bqhd,bkd->bqhk', qib, k_idx).astype(jnp.float32)
        score = jnp.einsum('bqh,bqhk->bqk', wb, jax.nn.relu(logits))
        chunk_q = (start + jnp.arange(Q_BLOCK)) // CHUNK
        adm = chunk_k[None, :] <= chunk_q[:, None]
        score = jnp.where(adm[None], score, NEG)
        _, sel = lax.top_k(score, topk)
        sel_ok = chunk_k[sel] <= chunk_q[None, :, None]
        k_sel = jax.vmap(lambda kk, ii: kk[ii])(k, sel)
        v_sel = jax.vmap(lambda vv, ii: vv[ii])(v, sel)
        s = jnp.einsum('bqhd,bqkd->bhqk', qb, k_sel).astype(jnp.float32) * scale
        s = jnp.where(sel_ok[:, None], s, NEG)
        p = jax.nn.softmax(s, axis=-1).astype(v.dtype)
        return jnp.einsum('bhqk,bqkd->bqhd', p, v_sel)

    return _blocks_to_seq(lax.map(one_block, jnp.arange(S // Q_BLOCK)))


def _hybrid_mixer(h, w_in, b_gate, rel_table, b_forget, w_branch, w_out, cos, sin):
    B_, S, _ = h.shape
    z = h @ w_in
    offs = np.cumsum(np.array(COL_SIZES))[:-1].tolist()
    (qa, ka, va, qb, kb, vb, fb, qc, kc, vc, qi, ki, wi, zg) = jnp.split(z, offs, axis=-1)
    heads = lambda t, n: t.reshape(B_, S, n, -1)

    ya = _chunk_band_attention(heads(qa, A_HEADS), heads(ka, A_HEADS), heads(va, A_HEADS), rel_table)

    log_f = jax.nn.log_sigmoid(fb.astype(jnp.float32) + b_forget.astype(jnp.float32))
    yb = _forgetting_attention(heads(qb, B_HEADS), heads(kb, B_HEADS), heads(vb, B_HEADS), log_f)

    qc_r = _rope(heads(qc, C_HEADS), cos, sin)
    kc_r = _rope(kc[:, :, None, :], cos, sin)[:, :, 0, :]
    qi_r = _rope(heads(qi, IDX_HEADS), cos, sin) * (IDX_DIM ** -0.5)
    ki_r = _rope(ki[:, :, None, :], cos, sin)[:, :, 0, :]
    wi_s = wi * (IDX_HEADS ** -0.5)
    yc = _indexed_sparse_attention(qc_r, kc_r, vc, qi_r, ki_r, wi_s)

    gates = jax.nn.sigmoid(zg.reshape(B_, S, N_BRANCH, D_MODEL) + b_gate)
    merged = (gates[:, :, 0] * (ya.reshape(B_, S, MIX_W) @ w_branch[0])
              + gates[:, :, 1] * (yb.reshape(B_, S, MIX_W) @ w_branch[1])
              + gates[:, :, 2] * (yc.reshape(B_, S, MIX_W) @ w_branch[2]))
    return merged @ w_out


def _conv_ffn(h, w_up, conv_w, conv_b, w_down):
    a, g = jnp.split(h @ w_up, 2, axis=-1)
    a = lax.conv_general_dilated(a, conv_w[:, None, :], window_strides=(1,),
                                 padding=[(CONV_W - 1, 0)],
                                 dimension_numbers=('NWC', 'WIO', 'NWC'),
                                 feature_group_count=D_FF) + conv_b
    return (jax.nn.gelu(a) * g) @ w_down


def setup_inputs(seed: int = 0) -> dict:
    key = jax.random.key(seed)
    ks = jax.random.split(key, 16)
    f32 = jnp.float32
    nrm = lambda k, shape, s: jax.random.normal(k, shape, f32) * s
    return {
        'x': nrm(ks[0], (BATCH, SEQ, D_MODEL), 1.0),
        'c': nrm(ks[1], (BATCH, D_MODEL), 1.0),
        'w_ada': nrm(ks[2], (DEPTH, D_MODEL, 6 * D_MODEL), D_MODEL ** -0.5),
        'b_ada': nrm(ks[3], (DEPTH, 6 * D_MODEL), 0.01),
        'norm_g': 1.0 + nrm(ks[4], (DEPTH, 4, D_MODEL), 0.05),
        'w_in': nrm(ks[5], (DEPTH, D_MODEL, N_IN), D_MODEL ** -0.5),
        'b_gate': nrm(ks[6], (DEPTH, N_BRANCH, D_MODEL), 0.01),
        'rel_table': nrm(ks[7], (DEPTH, A_HEADS, 2 * A_MAX_REL + 1), 0.2),
        'b_forget': jax.random.uniform(ks[8], (DEPTH, B_HEADS), f32, 1.0, 4.0),
        'w_branch': nrm(ks[9], (DEPTH, N_BRANCH, MIX_W, D_MODEL), MIX_W ** -0.5),
        'w_out': nrm(ks[10], (DEPTH, D_MODEL, D_MODEL), D_MODEL ** -0.5),
        'w_up': nrm(ks[11], (DEPTH, D_MODEL, 2 * D_FF), D_MODEL ** -0.5),
        'conv_w': nrm(ks[12], (DEPTH, CONV_W, D_FF), CONV_W ** -0.5),
        'conv_b': nrm(ks[13], (DEPTH, D_FF), 0.01),
        'w_down': nrm(ks[14], (DEPTH, D_FF, D_MODEL), D_FF ** -0.5),
    }


def reference(x, c, w_ada, b_ada, norm_g, w_in, b_gate, rel_table, b_forget, w_branch, w_out,
              w_up, conv_w, conv_b, w_down):
    S = x.shape[1]
    pos = jnp.arange(S, dtype=jnp.float32)
    inv_freq = ROPE_THETA ** (-jnp.arange(0, HEAD_DIM, 2, dtype=jnp.float32) / HEAD_DIM)
    ang = pos[:, None] * inv_freq[None, :]
    cos, sin = jnp.cos(ang), jnp.sin(ang)
    c_act = jax.nn.silu(c)
    for l in range(DEPTH):
        mod = (c_act @ w_ada[l] + b_ada[l])[:, None, :]
        sh1, sc1, g1, sh2, sc2, g2 = jnp.split(mod, 6, axis=-1)
        h = _rmsnorm(x, norm_g[l, 0]) * (1.0 + sc1) + sh1
        y = _hybrid_mixer(h, w_in[l], b_gate[l], rel_table[l], b_forget[l], w_branch[l], w_out[l], cos, sin)
        x = x + g1 * _rmsnorm(y, norm_g[l, 1])
        h = _rmsnorm(x, norm_g[l, 2]) * (1.0 + sc2) + sh2
        y = _conv_ffn(h, w_up[l], conv_w[l], conv_b[l], w_down[l])
        x = x + g2 * _rmsnorm(y, norm_g[l, 3])
    return x
```

```python
import numpy as np
from contextlib import ExitStack
import concourse.bass as bass
import concourse.mybir as mybir
from concourse.bass_utils import run_bass_kernel_spmd

F32 = mybir.dt.float32
BF16 = mybir.dt.bfloat16
AF = mybir.ActivationFunctionType
ALU = mybir.AluOpType
AX = mybir.AxisListType

D = 1024
KC = 8
NIN = 5320
DFF = 2816
FC = 22
NEGM = -30000.0
NIT = 12
EPS = 1e-6
NCST = 128 * 3 + 512 + 512 + NIT

OFF = dict(qa=0, ka=256, va=512, qb=768, kb=1024, vb=1280, fb=1536, qc=1540, kc=1796, vc=1860,
           qi=1924, ki=2180, wi=2244, zg=2248)
DST = dict(qa=0, ka=256, qb=512, kb=768, va=1024, vb=1280, qc=1536, qi=1792,
           kc=2048, ki=2112, vc=2176, fb=2240, wi=2244)
WID = dict(qa=256, ka=256, qb=256, kb=256, va=256, vb=256, qc=256, qi=256, kc=64, ki=64, vc=64, fb=4, wi=4)
NQKV = 2248


class Buf:
    __slots__ = ("name", "w", "r", "rd", "sem", "cnt", "excl")

    def __init__(self, name=""):
        self.name = name
        self.excl = False
        self.w = None
        self.r = {}
        self.rd = []
        self.sem = None
        self.cnt = 0


class Prog:
    ENG = ("pe", "act", "dve", "pool", "sp")

    def __init__(self, nc, stack):
        self.nc = nc
        self.stack = stack
        self.ops = []
        self.emitted = 0
        self.esem = {e: stack.enter_context(nc.semaphore("es_" + e)) for e in ("pe", "act", "dve", "pool")}
        self.ecnt = {e: 0 for e in self.esem}
        self.seen = {e: {} for e in self.ENG}
        self.tok = Buf("tok")
        self.nsem = 0
        self.last_bar = None
        self.free_sems = []
        self.sem_bufs = []

    def dsem(self, buf, fresh=False):
        if buf.sem is None:
            if fresh:
                self.nsem += 1
                buf.sem = self.stack.enter_context(self.nc.semaphore("dq%d" % self.nsem))
                buf.cnt = 0
                return buf.sem
            if self.free_sems:
                buf.sem, buf.cnt = self.free_sems.pop()
            else:
                self.nsem += 1
                buf.sem = self.stack.enter_context(self.nc.semaphore("ds%d" % self.nsem))
                buf.cnt = 0
            self.sem_bufs.append(buf)
        return buf.sem

    def release_sems(self):
        for b in self.sem_bufs:
            self.free_sems.append((b.sem, b.cnt))
            b.sem = None
            for e in self.ENG:
                self.seen[e].pop(id(b), None)
        self.sem_bufs = []

    def op(self, eng, fn, r=(), w=(), dma=None, tok=True):
        deps = set()
        w = list(w) + [b for b in r if b.excl and b not in w]
        r = [b for b in r if not b.excl]
        if tok:
            r.append(self.tok)
        for b in r:
            if b.w is not None:
                deps.add(b.w)
        for b in w:
            if b.w is not None:
                wo = self.ops[b.w]
                if not (dma is not None and wo["dma"] is dma and wo["eng"] == eng):
                    deps.add(b.w)
            deps.update(b.r.values())
            deps.update(b.rd)
        if tok and self.last_bar is not None:
            deps = {d for d in deps if d >= self.last_bar}
        i = len(self.ops)
        o = dict(eng=eng, fn=fn, deps=deps, dma=dma, needed=False, waits=None, cnt=None, dval=None)
        if dma is not None:
            self.dsem(dma, fresh=(eng == "pool"))
            dma.cnt += 16
            o["dval"] = dma.cnt
        self.ops.append(o)
        for b in r:
            if dma is not None:
                b.rd.append(i)
            else:
                b.r[eng] = i
        for b in w:
            b.w = i
            b.r = {}
            b.rd = []
        return i

    def barrier(self):
        self.last_bar = self.op("dve", lambda e: e.memset(self.bar_tile, 0.0), w=[self.tok], tok=False)
        self.ops[self.last_bar]["needed"] = True

    def emit(self):
        nc = self.nc
        ops = self.ops[self.emitted:]
        for o in ops:
            eng = o["eng"]
            seen = self.seen[eng]
            per = {}
            dmaw = {}
            for d in o["deps"]:
                do = self.ops[d]
                if do["dma"] is not None:
                    key = id(do["dma"])
                    if do["dval"] > seen.get(key, 0):
                        if key not in dmaw or dmaw[key][1] < do["dval"]:
                            dmaw[key] = (do["dma"], do["dval"])
                else:
                    f = do["eng"]
                    if f == "pe" and eng == "pe":
                        continue
                    if d > seen.get(f, -1):
                        per[f] = max(per.get(f, -1), d)
            w = []
            for f, d in per.items():
                seen[f] = d
                self.ops[d]["needed"] = True
                w.append(("e", f, d))
            for key, (buf, val) in dmaw.items():
                seen[key] = val
                w.append(("d", buf, val))
            o["waits"] = w
        for o in ops:
            if o["dma"] is None and o["needed"]:
                self.ecnt[o["eng"]] += 1
                o["cnt"] = self.ecnt[o["eng"]]
        per_eng = {e: [] for e in self.ENG}
        for o in ops:
            per_eng[o["eng"]].append(o)

        def run(eng_name):
            def body(e):
                for o in per_eng[eng_name]:
                    for wt in o["waits"]:
                        if wt[0] == "e":
                            e.wait_ge(self.esem[wt[1]], self.ops[wt[2]]["cnt"])
                        else:
                            e.wait_ge(wt[1].sem, wt[2])
                    if o["fn"] is None:
                        continue
                    ins = o["fn"](e)
                    if o["dma"] is not None:
                        ins.then_inc(o["dma"].sem, 16)
                    elif o["needed"]:
                        ins.then_inc(self.esem[eng_name], 1)
            return body

        with nc.Block() as block:
            block.tensor(run("pe"))
            block.scalar(run("act"))
            block.vector(run("dve"))
            block.gpsimd(run("pool"))
            block.sync(run("sp"))
        self.emitted = len(self.ops)
        if self.last_bar == len(self.ops) - 1:
            self.release_sems()


class T:
    def __init__(self, ap, name=""):
        self.ap = ap
        self.b = Buf(name)

    def __getitem__(self, k):
        return self.ap[k]


def build(cfg):
    S = cfg["S"]
    NSEQ = cfg["NSEQ"]
    NL = cfg["NL"]
    NT = S // 128
    GD = 256
    TOPK = min(256, S // 4)
    stop_after = cfg.get("stop_after", "D")
    dbg = cfg.get("dbg", False)
    skip = cfg.get("skip", "")

    nc = bass.Bass("TRN2", target_bir_lowering=False)

    def din(name, shape, dt=F32):
        return nc.dram_tensor(name, list(shape), dt, kind="ExternalInput").ap()

    x_d = din("x", [NSEQ, S, D])
    cT_d = din("cT", [128, KC, NSEQ])
    w_ada_d = din("w_ada", [2, D, 6 * D])
    b_adaT_d = din("b_adaT", [128, 2, 48])
    b_adaB_d = din("b_adaB", [128, 2, 2, D])
    normgT_d = din("normgT", [128, 2, 4, KC])
    normgB_d = din("normgB", [128, 2, 2, D])
    w_in_d = din("w_in", [2, D, NIN])
    b_gateT_d = din("b_gateT", [128, 2, 3, KC])
    biasT_d = din("biasT", [2, 128, 5 * 512])
    maskA_d = din("maskA", [128, 5 * 512])
    b_forgetB_d = din("b_forgetB", [128, 2, 4])
    w_branch_d = din("w_branch", [2, 3, 256, D])
    w_out_d = din("w_out", [2, D, D])
    w_up_d = din("w_up", [2, D, 2 * DFF])
    conv_wT_d = din("conv_wT", [128, 2, 3, FC])
    conv_bT_d = din("conv_bT", [128, 2, FC])
    w_down_d = din("w_down", [2, DFF, D])
    cs_d = din("cs", [NT, 128, 128])
    cst_d = din("cst", [128, NCST])
    out_d = nc.dram_tensor("out", [NSEQ, S, D], F32, kind="ExternalOutput").ap()
    xs_d = [nc.dram_tensor("xs%d" % i, [NSEQ, S, D], F32, kind="Internal").ap() for i in range(2)]
    yT_d = nc.dram_tensor("yT", [NSEQ, 128, 6, S], BF16, kind="ExternalOutput" if dbg else "Internal").ap()

    stack = ExitStack()
    with stack:
        pg = Prog(nc, stack)

        uid = [0]

        def sb(name, shape, dt, st=stack):
            uid[0] += 1
            return T(st.enter_context(nc.sbuf_tensor("%s_%d" % (name, uid[0]), list(shape), dt)), name)

        def MM(out, lhsT, rhs, start, stop, r, w):
            pg.op("pe", lambda e: e.matmul(out=out, lhsT=lhsT, rhs=rhs, start=start, stop=stop,
                                           skip_group_check=True), r=r, w=w)

        def TR(out, in_, ident, r, w):
            pg.op("pe", lambda e: e.transpose(out=out, in_=in_, identity=ident), r=r, w=w)

        def ACT(out, in_, func, r, w, bias=None, scale=None, accum_out=None):
            kw = {}
            if bias is not None:
                kw["bias"] = bias
            if scale is not None:
                kw["scale"] = scale
            if accum_out is not None:
                kw["accum_out"] = accum_out
            pg.op("act", lambda e: e.activation(out=out, in_=in_, func=func, **kw), r=r, w=w)

        def TS(eng, out, in0, s1, s2, op0, op1, r, w, accum_out=None):
            kw = {}
            if op1 is not None:
                kw["op1"] = op1
            if accum_out is not None:
                kw["accum_out"] = accum_out
            pg.op(eng, lambda e: e.tensor_scalar(out=out, in0=in0, scalar1=s1, scalar2=s2, op0=op0, **kw), r=r, w=w)

        def TT(eng, out, in0, in1, op, r, w):
            pg.op(eng, lambda e: e.tensor_tensor(out=out, in0=in0, in1=in1, op=op), r=r, w=w)

        def STT(eng, out, in0, scalar, in1, op0, op1, r, w):
            pg.op(eng, lambda e: e.scalar_tensor_tensor(out=out, in0=in0, scalar=scalar, in1=in1, op0=op0, op1=op1), r=r, w=w)

        def CP(eng, out, in_, r, w):
            if eng == "act":
                pg.op("act", lambda e: e.copy(out=out, in_=in_), r=r, w=w)
            else:
                pg.op(eng, lambda e: e.tensor_copy(out=out, in_=in_), r=r, w=w)

        def MS(eng, ap, val, w):
            pg.op(eng, lambda e: e.memset(ap, val), w=w)

        def DMA(eng, out, in_, r, w, sem):
            pg.op(eng, lambda e: e.dma_start(out=out, in_=in_), r=r, w=w, dma=sem)

        bar = sb("bar", [128, 8], F32)
        pg.bar_tile = bar[:, 0:1]
        PS = [T(stack.enter_context(nc.psum_tensor("ps%d" % i, [128, 512], F32)), "ps%d" % i) for i in range(8)]
        PSB = [p[:, :].bitcast(BF16) for p in PS]
        for p in PS:
            p.b.excl = True

        cst = sb("cst", [128, NCST], F32)
        identF = cst[:, 0:128]
        triF = cst[:, 128:256]
        sel127 = cst[:, 256:384]
        i4F = cst[:, 384:896]
        tribF = cst[:, 896:1408]
        pow2 = cst[:, 1408:1408 + NIT]
        identB = sb("identB", [128, 128], BF16)
        i4B = sb("i4B", [128, 512], BF16)
        tribB = sb("tribB", [128, 512], BF16)
        DMA("sp", cst[:, :], cst_d[:, :], [], [cst.b], cst.b)
        CP("dve", identB[:, :], identF, [cst.b], [identB.b])
        CP("dve", i4B[:, :], i4F, [cst.b], [i4B.b])
        CP("dve", tribB[:, :], tribF, [cst.b], [tribB.b])

        NSM = 96 + 64 + 48 + 8 + 6 * FC + 2 * FC + KC * NSEQ
        small = sb("small", [128, NSM], F32)
        o = 0
        b_adaT = small[:, o:o + 96].rearrange("p (l c) -> p l c", l=2); o += 96
        normgT = small[:, o:o + 64].rearrange("p (l j k) -> p l j k", l=2, j=4); o += 64
        b_gateT = small[:, o:o + 48].rearrange("p (l j k) -> p l j k", l=2, j=3); o += 48
        b_forgetB = small[:, o:o + 8].rearrange("p (l h) -> p l h", l=2); o += 8
        conv_wT = small[:, o:o + 6 * FC].rearrange("p (l j f) -> p l j f", l=2, j=3); o += 6 * FC
        conv_bT = small[:, o:o + 2 * FC].rearrange("p (l f) -> p l f", l=2); o += 2 * FC
        cT = small[:, o:o + KC * NSEQ].rearrange("p (k b) -> p k b", k=KC); o += KC * NSEQ
        for dst, src in ((b_adaT, b_adaT_d), (normgT, normgT_d), (b_gateT, b_gateT_d), (b_forgetB, b_forgetB_d),
                         (conv_wT, conv_wT_d), (conv_bT, conv_bT_d), (cT, cT_d)):
            DMA("sp", dst, src, [], [small.b], small.b)
        cact = sb("cact", [128, KC, NSEQ], F32)
        csig = sb("csig", [128, KC, NSEQ], F32)
        ACT(csig[:, :, :], cT, AF.Sigmoid, [small.b], [csig.b])
        TT("dve", cact[:, :, :], cT, csig[:, :, :], ALU.mult, [small.b, csig.b], [cact.b])
        modT = sb("modT", [128, NL, NSEQ, 4, KC], F32)
        G_d = nc.dram_tensor("Gscr", [NL, NSEQ, 2, 128, D], F32, kind="Internal").ap()
        biasAll = sb("biasAll", [128, NL, 5 * 512], BF16)

        with ExitStack() as st0:
            wad = [sb("wad%d" % i, [128, KC, 1024], F32, st0) for i in range(2)]
            cactB = sb("cactB", [128, NSEQ, KC, 128], F32, st0)
            gtl = [sb("gtl%d" % i, [128, D], F32, st0) for i in range(2)]
            biasF = sb("biasF", [128, 5 * 512], F32, st0)
            maskF = sb("maskF", [128, 5 * 512], F32, st0)
            DMA("sp", maskF[:, :], maskA_d[:, :], [], [maskF.b], maskF.b)
            for l in range(NL):
                DMA("sp", biasF[:, :], biasT_d[l], [], [biasF.b], biasF.b)
                TT("dve", biasAll[:, l, :], biasF[:, :], maskF[:, :], ALU.add, [biasF.b, maskF.b], [biasAll.b])
            for s in range(NSEQ):
                CP("dve", cactB[:, s, :, :], cact[:, :, s:s + 1].to_broadcast([128, KC, 128]), [cact.b], [cactB.b])
            gi = 0
            badaB = sb("badaB", [128, D], F32, st0)
            gB = sb("gB", [128, D], F32, st0)
            mtmp = sb("mtmp", [128, NSEQ], F32, st0)
            for l in range(NL):
                for sec in range(6):
                    wt = wad[(l * 6 + sec) % 2]
                    src = w_ada_d[l].rearrange("(k p) n -> p k n", p=128)[:, :, sec * 1024:(sec + 1) * 1024]
                    for k0 in range(0, KC, 2):
                        DMA("sp", wt[:, k0:k0 + 2, :], src[:, k0:k0 + 2, :], [], [wt.b], wt.b)
                    if sec in (2, 5):
                        j = 0 if sec == 2 else 1
                        DMA("sp", badaB[:, :], b_adaB_d[:, l, j, :], [], [badaB.b], badaB.b)
                        DMA("sp", gB[:, :], normgB_d[:, l, j, :], [], [gB.b], gB.b)
                        for s in range(NSEQ):
                            g = gtl[gi % 2]
                            gi += 1
                            for half in range(2):
                                ps = PS[half]
                                for k in range(KC):
                                    MM(ps[:, :], cactB[:, s, k, :], wt[:, k, half * 512:(half + 1) * 512],
                                       k == 0, k == KC - 1, [cactB.b, wt.b], [ps.b])
                                hs = slice(half * 512, (half + 1) * 512)
                                TT("dve", g[:, hs], ps[:, :], badaB[:, hs], ALU.add, [ps.b, badaB.b], [g.b])
                                TT("dve", g[:, hs], g[:, hs], gB[:, hs], ALU.mult, [g.b, gB.b], [g.b])
                            DMA("sp", G_d[l, s, j], g[:, :], [g.b], [], g.b)
                    else:
                        jj = {0: 1, 1: 0, 3: 3, 4: 2}[sec]
                        for kc in range(KC):
                            ps = PS[2 + kc % 2]
                            for k in range(KC):
                                MM(ps[:, 0:NSEQ], wt[:, k, kc * 128:(kc + 1) * 128], cact[:, k, :],
                                   k == 0, k == KC - 1, [cact.b, wt.b], [ps.b])
                            cc = sec * 8 + kc
                            if sec in (0, 3):
                                TS("dve", modT[:, l, :, jj, kc], ps[:, 0:NSEQ], b_adaT[:, l, cc:cc + 1], None, ALU.add, None,
                                   [ps.b, small.b], [modT.b])
                            else:
                                ng = 0 if sec == 1 else 2
                                TS("dve", mtmp[:, :], ps[:, 0:NSEQ], b_adaT[:, l, cc:cc + 1], 1.0, ALU.add, ALU.add,
                                   [ps.b, small.b], [mtmp.b])
                                TS("dve", modT[:, l, :, jj, kc], mtmp[:, :], normgT[:, l, ng, kc:kc + 1], None, ALU.mult, None,
                                   [mtmp.b, small.b], [modT.b])
            pg.barrier()
            pg.emit()

        def norm_hT(xt, xn, junk, ss, hT_ap_fn, A, Bv, psA, psB, extra_r=()):
            ACT(junk[:, 0:D], xt[:, :], AF.Square, [xt.b], [junk.b, ss.b], accum_out=ss[:, 0:1])
            ACT(ss[:, 1:2], ss[:, 0:1], AF.Sqrt, [ss.b], [ss.b], bias=EPS, scale=1.0 / D)
            pg.op("dve", lambda e: e.reciprocal(out=ss[:, 2:3], in_=ss[:, 1:2]), r=[ss.b], w=[ss.b])
            ACT(xn[:, :], xt[:, :], AF.Copy, [xt.b, ss.b], [xn.b], scale=ss[:, 2:3])
            for half, ps in ((0, psA), (1, psB)):
                for kk in range(4):
                    k = half * 4 + kk
                    TR(ps[:, kk * 128:(kk + 1) * 128], xn[:, k * 128:(k + 1) * 128], identF, [xn.b, cst.b], [ps.b])
                for kk in range(4):
                    k = half * 4 + kk
                    outap, wb = hT_ap_fn(k)
                    if kk % 2 == 0:
                        ACT(outap, ps[:, kk * 128:(kk + 1) * 128], AF.Identity, [ps.b, modT.b], [wb],
                            bias=Bv[:, k:k + 1], scale=A[:, k:k + 1])
                    else:
                        TS("dve", outap, ps[:, kk * 128:(kk + 1) * 128], A[:, k:k + 1], Bv[:, k:k + 1], ALU.mult, ALU.add,
                           [ps.b, modT.b], [wb])

        def postnorm_residual(psA, psB, xres, G, ss, ot, junk):
            ACT(junk[:, 0:512], psA[:, :], AF.Square, [psA.b], [junk.b, ss.b], accum_out=ss[:, 4:5])
            ACT(junk[:, 512:1024], psB[:, :], AF.Square, [psB.b], [junk.b, ss.b], accum_out=ss[:, 5:6])
            TT("dve", ss[:, 6:7], ss[:, 4:5], ss[:, 5:6], ALU.add, [ss.b], [ss.b])
            ACT(ss[:, 3:4], ss[:, 6:7], AF.Sqrt, [ss.b], [ss.b], bias=EPS, scale=1.0 / D)
            pg.op("dve", lambda e: e.reciprocal(out=ss[:, 7:8], in_=ss[:, 3:4]), r=[ss.b], w=[ss.b])
            for half, ps in ((0, psA), (1, psB)):
                hs = slice(half * 512, (half + 1) * 512)
                STT("dve", ot[:, hs], ps[:, :], ss[:, 7:8], G[:, hs], ALU.mult, ALU.mult, [ps.b, ss.b, G.b], [ot.b])
                TT("pool", ot[:, hs], ot[:, hs], xres[:, hs], ALU.add, [ot.b, xres.b], [ot.b])

        def interleave(g1, g2):
            d1 = g1 is None
            d2 = g2 is None
            while not (d1 and d2):
                if not d1:
                    try:
                        next(g1)
                    except StopIteration:
                        d1 = True
                if not d2:
                    try:
                        next(g2)
                    except StopIteration:
                        d2 = True

        for l in range(NL if stop_after != "0" else 0):
            xin_d = x_d if l == 0 else xs_d[1]
            x1_d = xs_d[0]
            xout_d = out_d if l == NL - 1 else xs_d[1]
            for s in range(NSEQ):
                A1 = modT[:, l, s, 0, :]
                B1 = modT[:, l, s, 1, :]
                A2 = modT[:, l, s, 2, :]
                B2 = modT[:, l, s, 3, :]
                with ExitStack() as st:
                    wq = sb("wq", [128, KC, NQKV], BF16, st)
                    KaT = sb("KaT", [64, 4, 1024], BF16, st)
                    Va = sb("Va", [128, 8, 4, 65], BF16, st)
                    KbT = sb("KbT", [70, 4, S], BF16, st)
                    Vb = sb("Vb", [128, NT, 4, 65], BF16, st)
                    KcT = sb("KcT", [64, S], BF16, st)
                    KiT = sb("KiT", [64, S], BF16, st)
                    Vc = sb("Vc", [128, NT, 65], BF16, st)
                    kcb = [Buf("kc%d" % i) for i in range(NT)]
                    kib = [Buf("ki%d" % i) for i in range(NT)]
                    vcb = [Buf("vc%d" % i) for i in range(NT)]
                    xts = [sb("xt%d" % i, [128, D], F32, st) for i in range(2)]
                    xn = sb("xn", [128, D], F32, st)
                    hT = sb("hT", [128, KC, 128], BF16, st)
                    css = [sb("cs%d" % i, [128, 128], F32, st) for i in range(2)]
                    zqa = sb("zqa", [128, 256], BF16, st)
                    zka = sb("zka", [128, 256], BF16, st)
                    zqb = sb("zqb", [128, 4, 70], BF16, st)
                    zkb = sb("zkb", [128, 4, 70], BF16, st)
                    zrq = sb("zrq", [128, 8, 64], BF16, st)
                    zrk = sb("zrk", [128, 2, 64], BF16, st)
                    rt1 = sb("rt1", [128, 8, 32], F32, st)
                    rt2 = sb("rt2", [128, 8, 32], F32, st)
                    QaT = sb("QaT", [64, 4, 128], BF16, st)
                    QbT = sb("QbT", [70, 4, 128], BF16, st)
                    QcTs = [sb("QcT%d" % i, [64, 4, 128], BF16, st) for i in range(2)]
                    QiTs = [sb("QiT%d" % i, [64, 4, 128], BF16, st) for i in range(2)]
                    fos = [sb("fo%d" % i, [128, 64], F32, st) for i in range(2)]
                    fob = sb("fob", [128, 16], BF16, st)
                    cum = [sb("cum%d" % i, [128, 4], F32, st) for i in range(2)]
                    ss = sb("ss", [128, 8], F32, st)
                    bis = sb("bis", [128, 8 + NIT], F32, st)
                    score = sb("score", [128, S], F32, st)
                    MBt = sb("MBt", [128, S], BF16, st)
                    junk = sb("junk", [128, D], BF16, st)
                    rts = [sb("rts%d" % i, [128, 512], F32, st) for i in range(2)]
                    pTs1 = [sb("pTa%d" % i, [128, 512], BF16, st) for i in range(5)]
                    pTs2 = [sb("pTb%d" % i, [128, 512], BF16, st) for i in range(3)]
                    yts = [sb("yt%d" % i, [128, 768], BF16, st) for i in range(2)]
                    rinv1 = sb("rinv1", [128, 4], F32, st)
                    rinv2 = sb("rinv2", [128, 4], F32, st)
                    yTs = sb("yTs", [128, 6, 128], BF16, st)

                    wsrc = w_in_d[l].rearrange("(k p) n -> p k n", p=128)
                    for nm in ("qa", "ka", "qb", "kb", "va", "vb", "qc", "qi", "kc", "ki", "vc", "fb", "wi"):
                        DMA("pool", wq[:, :, DST[nm]:DST[nm] + WID[nm]], wsrc[:, :, OFF[nm]:OFF[nm] + WID[nm]], [], [wq.b], wq.b)
                    MS("dve", Va[:, :, :, 64:65], 1.0, [Va.b])
                    MS("dve", Vb[:, :, :, 64:65], 1.0, [Vb.b])
                    MS("dve", Vc[:, :, 64:65], 1.0, vcb)
                    MS("dve", zqb[:, :, 67:70], 1.0, [zqb.b])
                    MS("dve", zkb[:, :, 64:67], 1.0, [zkb.b])
                    MS("dve", cum[1][:, :], 0.0, [cum[1].b])

                    cnt1 = [0, 0]
                    cnt2 = [0, 0]

                    def recip_norm(acc, rinv, ydst):
                        accv = acc[:, 0:260].rearrange("p (h d) -> p h d", h=4)
                        pg.op("dve", lambda e: e.reciprocal(out=rinv[:, :], in_=accv[:, :, 64]), r=[acc.b], w=[rinv.b])
                        return accv

                    def stage1(t):
                        xt = xts[t % 2]
                        cs = css[t % 2]
                        fo = fos[t % 2]
                        QcT = QcTs[t % 2]
                        QiT = QiTs[t % 2]
                        yt = yts[t % 2]
                        DMA("sp", xt[:, :], xin_d[s, t * 128:(t + 1) * 128, :], [], [xt.b], xt.b)
                        DMA("sp", cs[:, :], cs_d[t], [], [cs.b], cs.b)
                        norm_hT(xt, xn, junk, ss, lambda k: (hT[:, k, :], hT.b), A1, B1, PS[0], PS[7])
                        yield
                        cosk = cs[:, 0:32]
                        sink = cs[:, 32:64]
                        cosq = cs[:, 64:96]
                        sinq = cs[:, 96:128]
                        blocks = [(0, 512), (512, 512), (1024, 512), (1536, 512), (2048, 200)]
                        for bi, (c0, cw) in enumerate(blocks):
                            ps = PS[0] if bi % 2 == 0 else PS[7]
                            for k in range(KC):
                                MM(ps[:, 0:cw], hT[:, k, :], wq[:, k, c0:c0 + cw], k == 0, k == KC - 1, [hT.b, wq.b], [ps.b])
                            if bi == 0:
                                TS("dve", zqa[:, :], ps[:, 0:256], 0.125, None, ALU.mult, None, [ps.b], [zqa.b])
                                CP("act", zka[:, :], ps[:, 256:512], [ps.b], [zka.b])
                            elif bi == 1:
                                TS("dve", zqb[:, :, 0:64], ps[:, 0:256].rearrange("p (h d) -> p h d", h=4), 0.125, None,
                                   ALU.mult, None, [ps.b], [zqb.b])
                                CP("act", zkb[:, :, 0:64], ps[:, 256:512].rearrange("p (h d) -> p h d", h=4), [ps.b], [zkb.b])
                            elif bi == 2:
                                CP("act", Va[:, t % 8, :, 0:64], ps[:, 0:256].rearrange("p (h d) -> p h d", h=4), [ps.b], [Va.b])
                                CP("dve", Vb[:, t, :, 0:64], ps[:, 256:512].rearrange("p (h d) -> p h d", h=4), [ps.b], [Vb.b])
                            elif bi == 3:
                                z3 = ps[:, 0:512].rearrange("p (h d) -> p h d", h=8)
                                x1 = z3[:, :, 0:32]
                                x2 = z3[:, :, 32:64]
                                cb = cosq.unsqueeze(1).to_broadcast([128, 8, 32])
                                sbb = sinq.unsqueeze(1).to_broadcast([128, 8, 32])
                                TT("dve", rt1[:, :, :], x1, cb, ALU.mult, [ps.b, cs.b], [rt1.b])
                                TT("dve", rt2[:, :, :], x2, sbb, ALU.mult, [ps.b, cs.b], [rt2.b])
                                TT("pool", zrq[:, :, 0:32], rt1[:, :, :], rt2[:, :, :], ALU.subtract, [rt1.b, rt2.b], [zrq.b])
                                TT("dve", rt1[:, :, :], x1, sbb, ALU.mult, [ps.b, cs.b], [rt1.b])
                                TT("dve", rt2[:, :, :], x2, cb, ALU.mult, [ps.b, cs.b], [rt2.b])
                                TT("pool", zrq[:, :, 32:64], rt1[:, :, :], rt2[:, :, :], ALU.add, [rt1.b, rt2.b], [zrq.b])
                            else:
                                z4 = ps[:, 0:128].rearrange("p (h d) -> p h d", h=2)
                                x1 = z4[:, :, 0:32]
                                x2 = z4[:, :, 32:64]
                                cb = cosk.unsqueeze(1).to_broadcast([128, 2, 32])
                                sbb = sink.unsqueeze(1).to_broadcast([128, 2, 32])
                                TT("dve", rt1[:, 0:2, :], x1, cb, ALU.mult, [ps.b, cs.b], [rt1.b])
                                TT("dve", rt2[:, 0:2, :], x2, sbb, ALU.mult, [ps.b, cs.b], [rt2.b])
                                TT("pool", zrk[:, :, 0:32], rt1[:, 0:2, :], rt2[:, 0:2, :], ALU.subtract, [rt1.b, rt2.b], [zrk.b])
                                TT("dve", rt1[:, 0:2, :], x1, sbb, ALU.mult, [ps.b, cs.b], [rt1.b])
                                TT("dve", rt2[:, 0:2, :], x2, cb, ALU.mult, [ps.b, cs.b], [rt2.b])
                                TT("pool", zrk[:, :, 32:64], rt1[:, 0:2, :], rt2[:, 0:2, :], ALU.add, [rt1.b, rt2.b], [zrk.b])
                                CP("act", Vc[:, t, 0:64], ps[:, 128:192], [ps.b], [vcb[t]])
                                TT("dve", fo[:, 0:4], ps[:, 192:196], b_forgetB[:, l, :], ALU.add, [ps.b, small.b], [fo.b])
                                ACT(fo[:, 16:20], ps[:, 196:200], AF.Abs, [ps.b], [fo.b], scale=0.5)
                                TS("dve", fo[:, 20:24], ps[:, 196:200], 0.0, 2.0, ALU.is_ge, ALU.mult, [ps.b], [fo.b])
                                TS("dve", fo[:, 20:24], fo[:, 20:24], -1.0, None, ALU.add, None, [fo.b], [fo.b])
                                ACT(fo[:, 4:8], fo[:, 0:4], AF.Exp, [fo.b], [fo.b], scale=-1.0)
                                ACT(fo[:, 8:12], fo[:, 4:8], AF.Ln, [fo.b], [fo.b], bias=1.0)
                            yield
                        cprev = cum[(t + 1) % 2]
                        ccur = cum[t % 2]
                        psc = PS[6]
                        MM(psc[:, 0:4], triF, fo[:, 8:12], True, False, [cst.b, fo.b], [psc.b])
                        MM(psc[:, 0:4], sel127, cprev[:, :], False, True, [cst.b, cprev.b], [psc.b])
                        CP("dve", ccur[:, :], psc[:, 0:4], [psc.b], [ccur.b])
                        CP("dve", fob[:, 0:4], ccur[:, :], [ccur.b], [fob.b])
                        TT("dve", fo[:, 24:28], ccur[:, :], fob[:, 0:4], ALU.subtract, [ccur.b, fob.b], [fo.b])
                        CP("dve", fob[:, 4:8], fo[:, 24:28], [fo.b], [fob.b])
                        TT("dve", fo[:, 28:32], fo[:, 24:28], fob[:, 4:8], ALU.subtract, [fo.b, fob.b], [fo.b])
                        CP("dve", fob[:, 8:12], fo[:, 28:32], [fo.b], [fob.b])
                        for i3 in range(3):
                            src3 = fob[:, 4 * i3:4 * i3 + 4].unsqueeze(2)
                            CP("dve", zkb[:, :, 67 + i3:68 + i3], src3, [fob.b], [zkb.b])
                            TS("dve", zqb[:, :, 64 + i3:65 + i3], src3, -1.0, None, ALU.mult, None, [fob.b], [zqb.b])
                        yield
                        pst = PSB[7]
                        pstb = PS[7].b
                        for h in range(4):
                            TR(pst[0:64, h * 128:(h + 1) * 128], zqa[:, h * 64:(h + 1) * 64], identB[:, :], [zqa.b, identB.b], [pstb])
                            TR(pst[0:64, 512 + h * 128:512 + (h + 1) * 128], zka[:, h * 64:(h + 1) * 64], identB[:, :], [zka.b, identB.b], [pstb])
                        CP("dve", QaT[:, :, :], pst[0:64, 0:512].rearrange("p (h q) -> p h q", h=4), [pstb], [QaT.b])
                        CP("act", KaT[:, :, (t % 8) * 128:(t % 8 + 1) * 128], pst[0:64, 512:1024].rearrange("p (h q) -> p h q", h=4), [pstb], [KaT.b])
                        yield
                        for h in range(4):
                            TR(pst[0:70, h * 128:(h + 1) * 128], zqb[:, h, :], identB[:, :], [zqb.b, identB.b], [pstb])
                            TR(pst[0:70, 512 + h * 128:512 + (h + 1) * 128], zkb[:, h, :], identB[:, :], [zkb.b, identB.b], [pstb])
                        CP("dve", QbT[:, :, :], pst[0:70, 0:512].rearrange("p (h q) -> p h q", h=4), [pstb], [QbT.b])
                        CP("act", KbT[:, :, t * 128:(t + 1) * 128], pst[0:70, 512:1024].rearrange("p (h q) -> p h q", h=4), [pstb], [KbT.b])
                        yield
                        for h in range(4):
                            TR(pst[0:64, h * 128:(h + 1) * 128], zrq[:, h, :], identB[:, :], [zrq.b, identB.b], [pstb])
                            TR(pst[0:64, 512 + h * 128:512 + (h + 1) * 128], zrq[:, 4 + h, :], identB[:, :], [zrq.b, identB.b], [pstb])
                        CP("dve", QcT[:, :, :], pst[0:64, 0:512].rearrange("p (h q) -> p h q", h=4), [pstb], [QcT.b])
                        CP("act", QiT[:, :, :], pst[0:64, 512:1024].rearrange("p (h q) -> p h q", h=4), [pstb], [QiT.b])
                        TR(pst[0:64, 0:128], zrk[:, 0, :], identB[:, :], [zrk.b, identB.b], [pstb])
                        TR(pst[0:64, 128:256], zrk[:, 1, :], identB[:, :], [zrk.b, identB.b], [pstb])
                        CP("dve", KcT[:, t * 128:(t + 1) * 128], pst[0:64, 0:128], [pstb], [kcb[t]])
                        CP("act", KiT[:, t * 128:(t + 1) * 128], pst[0:64, 128:256], [pstb], [kib[t]])
                        yield
                        acc = PS[6]

                        def nS():
                            cnt1[0] += 1
                            return PS[4 + cnt1[0] % 2]

                        def nP():
                            cnt1[1] += 1
                            return pTs1[cnt1[1] % 5]
                        if 'a' not in skip:
                            mlist = [m for m in range(5) if t - 4 + m >= 0]
                            pa = {}
                            for m in mlist:
                                sl = (t - 4 + m) % 8
                                sps = nS()
                                for h in range(4):
                                    MM(sps[:, h * 128:(h + 1) * 128], KaT[:, h, sl * 128:(sl + 1) * 128],
                                       QaT[:, h, :], h == 0, False, [KaT.b, QaT.b], [sps.b])
                                MM(sps[:, :], identB[:, :], biasAll[:, l, m * 512:(m + 1) * 512], False, True, [identB.b, biasAll.b], [sps.b])
                                p = nP()
                                ACT(p[:, :], sps[:, :], AF.Exp, [sps.b], [p.b])
                                pa[m] = p
                                yield
                            first = True
                            for h in range(4):
                                for m in mlist:
                                    sl = (t - 4 + m) % 8
                                    MM(acc[:, h * 65:(h + 1) * 65], pa[m][:, h * 128:(h + 1) * 128], Va[:, sl, h, :], first, False,
                                       [pa[m].b, Va.b], [acc.b])
                                    first = False
                            accv = recip_norm(acc, rinv1, yt)
                            TT("dve", yt[:, 0:256].rearrange("p (h d) -> p h d", h=4), accv[:, :, 0:64],
                               rinv1[:, :].unsqueeze(2).to_broadcast([128, 4, 64]), ALU.mult, [acc.b, rinv1.b], [yt.b])
                            yield
                        if 'b' not in skip:
                            def qk_b(j):
                                sps = nS()
                                for h in range(4):
                                    MM(sps[:, h * 128:(h + 1) * 128], KbT[:, h, j * 128:(j + 1) * 128], QbT[:, h, :], h == 0, False,
                                       [KbT.b, QbT.b], [sps.b])
                                if j == t:
                                    MM(sps[:, :], identB[:, :], tribB[:, :], False, True, [identB.b, tribB.b], [sps.b])
                                p = nP()
                                ACT(p[:, :], sps[:, :], AF.Exp, [sps.b], [p.b])
                                return p

                            def pv_b(j, p, first):
                                for h in range(4):
                                    MM(acc[:, h * 65:(h + 1) * 65], p[:, h * 128:(h + 1) * 128], Vb[:, j, h, :], first and h == 0, False,
                                       [p.b, Vb.b], [acc.b])
                            pprev = qk_b(0)
                            for j in range(1, t + 1):
                                pcur = qk_b(j)
                                pv_b(j - 1, pprev, j == 1)
                                pprev = pcur
                                yield
                            pv_b(t, pprev, t == 0)
                            yield
                            accv = recip_norm(acc, rinv1, yt)
                            TT("dve", yt[:, 256:512].rearrange("p (h d) -> p h d", h=4), accv[:, :, 0:64],
                               rinv1[:, :].unsqueeze(2).to_broadcast([128, 4, 64]), ALU.mult, [acc.b, rinv1.b], [yt.b])
                            yield

                    def stage2(t):
                        fo = fos[t % 2]
                        QcT = QcTs[t % 2]
                        QiT = QiTs[t % 2]
                        yt = yts[t % 2]
                        acc = PS[1]

                        def nS():
                            cnt2[0] += 1
                            return PS[2 + cnt2[0] % 2]

                        def nP():
                            cnt2[1] += 1
                            return pTs2[cnt2[1] % 3]
                        if 'c' not in skip:
                            N = 128 * (t + 1)
                            nkb = (N + 511) // 512
                            for kb in range(nkb):
                                cw = min(512, N - kb * 512)
                                kbufs = kib[kb * 4:min(kb * 4 + 4, t + 1)]
                                for h in range(4):
                                    sps = nS()
                                    MM(sps[:, 0:cw], QiT[:, h, :], KiT[:, kb * 512:kb * 512 + cw], True, True, [QiT.b] + kbufs, [sps.b])
                                    rt = rts[h % 2]
                                    ACT(rt[:, 0:cw], sps[:, 0:cw], AF.Relu, [sps.b, fo.b], [rt.b], scale=fo[:, 16 + h:17 + h])
                                    sc = score[:, kb * 512:kb * 512 + cw]
                                    if h == 0:
                                        TS("dve", sc, rt[:, 0:cw], fo[:, 20:21], None, ALU.mult, None, [rt.b, fo.b], [score.b])
                                    else:
                                        STT("dve", sc, rt[:, 0:cw], fo[:, 20 + h:21 + h], sc, ALU.mult, ALU.add, [rt.b, fo.b, score.b], [score.b])
                                    yield
                            if N <= TOPK or 'n' in skip:
                                MS("dve", bis[:, 0:1], -1e29, [bis.b])
                                MS("dve", score[0:64, t * 128 + 64:(t + 1) * 128], -1e30, [score.b])
                            else:
                                pg.op("dve", lambda e, N=N: e.tensor_reduce(out=bis[:, 1:2], in_=score[:, 0:N], axis=AX.X, op=ALU.max), r=[score.b], w=[bis.b])
                                pg.op("dve", lambda e, N=N: e.tensor_reduce(out=bis[:, 2:3], in_=score[:, 0:N], axis=AX.X, op=ALU.min), r=[score.b], w=[bis.b])
                                MS("dve", score[0:64, t * 128 + 64:(t + 1) * 128], -1e30, [score.b])
                                TT("dve", bis[:, 3:4], bis[:, 1:2], bis[:, 2:3], ALU.subtract, [bis.b], [bis.b])
                                TS("dve", bis[:, 8:8 + NIT], pow2, bis[:, 3:4], None, ALU.mult, None, [cst.b, bis.b], [bis.b])
                                TT("dve", bis[:, 4:5], bis[:, 2:3], bis[:, 8:9], ALU.add, [bis.b], [bis.b])
                                yield
                                for it in range(NIT):
                                    TS("dve", MBt[:, 0:N], score[:, 0:N], bis[:, 4:5], None, ALU.is_ge, ALU.add, [score.b, bis.b],
                                       [MBt.b, bis.b], accum_out=bis[:, 5:6])
                                    if it < NIT - 1:
                                        TS("dve", bis[:, 6:7], bis[:, 5:6], TOPK - 0.5, 0.5, ALU.is_ge, ALU.subtract, [bis.b], [bis.b])
                                        STT("dve", bis[:, 4:5], bis[:, 6:7], bis[:, 8 + it:9 + it], bis[:, 4:5], ALU.mult, ALU.add,
                                            [bis.b], [bis.b])
                                    else:
                                        TS("dve", bis[:, 6:7], bis[:, 5:6], TOPK - 0.5, 1.0, ALU.is_ge, ALU.subtract, [bis.b], [bis.b])
                                        STT("dve", bis[:, 0:1], bis[:, 6:7], bis[:, 8 + it:9 + it], bis[:, 4:5], ALU.mult, ALU.add,
                                            [bis.b], [bis.b])
                                    yield
                            TS("dve", MBt[:, 0:N], score[:, 0:N], bis[:, 0:1], NEGM, ALU.is_lt, ALU.mult, [score.b, bis.b], [MBt.b])
                            yield
                            def qk_c(j):
                                sps = nS()
                                MM(sps[:, :], KcT[:, j * 128:(j + 1) * 128], QcT[:, :, :].rearrange("p h q -> p (h q)"), True, False,
                                   [kcb[j], QcT.b], [sps.b])
                                MM(sps[:, :], MBt[:, j * 128:(j + 1) * 128], i4B[:, :], False, True, [MBt.b, i4B.b], [sps.b])
                                p = nP()
                                ACT(p[:, :], sps[:, :], AF.Exp, [sps.b], [p.b])
                                return p

                            def pv_c(j, p, first):
                                for h in range(4):
                                    MM(acc[:, h * 65:(h + 1) * 65], p[:, h * 128:(h + 1) * 128], Vc[:, j, :], first and h == 0, False,
                                       [p.b, vcb[j]], [acc.b])
                            pprev = qk_c(0)
                            for j in range(1, t + 1):
                                pcur = qk_c(j)
                                pv_c(j - 1, pprev, j == 1)
                                pprev = pcur
                                yield
                            pv_c(t, pprev, t == 0)
                            yield
                            accv = recip_norm(acc, rinv2, yt)
                            TT("dve", yt[:, 512:768].rearrange("p (h d) -> p h d", h=4), accv[:, :, 0:64],
                               rinv2[:, :].unsqueeze(2).to_broadcast([128, 4, 64]), ALU.mult, [acc.b, rinv2.b], [yt.b])
                        pst2 = PSB[3]
                        pst2b = PS[3].b
                        for c6 in range(6):
                            TR(pst2[:, c6 * 128:(c6 + 1) * 128], yt[:, c6 * 128:(c6 + 1) * 128], identB[:, :], [yt.b, identB.b], [pst2b])
                        CP("act", yTs[:, :, :], pst2[:, 0:768].rearrange("p (c q) -> p c q", c=6), [pst2b], [yTs.b])
                        DMA("sp", yT_d[s, :, :, t * 128:(t + 1) * 128], yTs[:, :, :], [yTs.b], [], yTs.b)
                        yield

                    def interleave(g1, g2):
                        d1 = g1 is None
                        d2 = g2 is None
                        while not (d1 and d2):
                            if not d1:
                                try:
                                    next(g1)
                                except StopIteration:
                                    d1 = True
                            if not d2:
                                try:
                                    next(g2)
                                except StopIteration:
                                    d2 = True

                    for t in range(NT):
                        interleave(stage1(t), stage2(t - 1) if t >= 1 else None)
                    interleave(None, stage2(NT - 1))
                    pg.barrier()
                    pg.emit()
                if stop_after == "AB":
                    continue
                GC = 512 if S % 512 == 0 else 256
                with ExitStack() as st:
                    wg = sb("wg", [128, KC, 3072], BF16, st)
                    wb = sb("wb", [128, 6, D], BF16, st)
                    wo = sb("wo", [128, KC, D], BF16, st)
                    NQ = GC // 128
                    xt4s = [[sb("xc%d_%d" % (j, i), [128, D], F32, st) for i in range(NQ)] for j in range(2)]
                    xn = sb("xn", [128, D], F32, st)
                    junk = sb("junk", [128, D], BF16, st)
                    hT4s = [sb("hT4_%d" % j, [128, KC, GC], BF16, st) for j in range(2)]
                    yT4s = [sb("yT4_%d" % j, [128, 6, GC], BF16, st) for j in range(2)]
                    gts = [sb("gt%d" % i, [128, GC], F32, st) for i in range(3)]
                    tm = [sb("tm%d" % i, [128, GC], F32, st) for i in range(2)]
                    mT = sb("mT", [128, KC, GC], BF16, st)
                    ots = [sb("ot%d" % i, [128, D], F32, st) for i in range(2)]
                    ss = sb("ss", [128, 8], F32, st)
                    ss2 = sb("ss2", [128, 8], F32, st)
                    junk2 = sb("junk2", [128, D], BF16, st)
                    wsrc = w_in_d[l].rearrange("(k p) n -> p k n", p=128)
                    for br in range(3):
                        DMA("pool", wg[:, :, br * 1024:(br + 1) * 1024], wsrc[:, :, OFF["zg"] + br * 1024:OFF["zg"] + (br + 1) * 1024],
                            [], [wg.b], wg.b)
                    DMA("pool", wb[:, :, :], w_branch_d[l].rearrange("b (k p) n -> p (b k) n", p=128), [], [wb.b], wb.b)
                    DMA("pool", wo[:, :, :], w_out_d[l].rearrange("(k p) n -> p k n", p=128), [], [wo.b], wo.b)
                    G1 = sb("G1", [128, D], F32, st)
                    DMA("sp", G1[:, :], G_d[l, s, 0], [], [G1.b], G1.b)
                    oic = [0]

                    def c_norm(g):
                        t0 = g * GC
                        hT4 = hT4s[g % 2]
                        yT4 = yT4s[g % 2]
                        DMA("sp", yT4[:, :, :], yT_d[s, :, :, t0:t0 + GC], [], [yT4.b], yT4.b)
                        for q in range(NQ):
                            xt = xt4s[g % 2][q]
                            DMA("sp", xt[:, :], xin_d[s, t0 + q * 128:t0 + (q + 1) * 128, :], [], [xt.b], xt.b)
                            norm_hT(xt, xn, junk, ss, (lambda q: lambda k: (hT4[:, k, q * 128:(q + 1) * 128], hT4.b))(q), A1, B1, PS[0], PS[1])
                            yield

                    def c_main(g):
                        t0 = g * GC
                        hT4 = hT4s[g % 2]
                        yT4 = yT4s[g % 2]
                        for fc in range(KC):
                            pps = []
                            for br in range(3):
                                ps = PS[2 + br]
                                for k in range(KC):
                                    MM(ps[:, 0:GC], wg[:, k, br * 1024 + fc * 128:br * 1024 + (fc + 1) * 128], hT4[:, k, :], k == 0, k == KC - 1,
                                       [wg.b, hT4.b], [ps.b])
                                ACT(gts[br][:, :], ps[:, 0:GC], AF.Sigmoid, [ps.b, small.b], [gts[br].b], bias=b_gateT[:, l, br, fc:fc + 1])
                                pp = PS[5 + br]
                                for kk in range(2):
                                    MM(pp[:, 0:GC], wb[:, br * 2 + kk, fc * 128:(fc + 1) * 128], yT4[:, br * 2 + kk, :], kk == 0, kk == 1,
                                       [wb.b, yT4.b], [pp.b])
                                pps.append(pp)
                            TT("dve", tm[0][:, :], gts[0][:, :], pps[0][:, 0:GC], ALU.mult, [gts[0].b, pps[0].b], [tm[0].b])
                            TT("dve", tm[1][:, :], gts[1][:, :], pps[1][:, 0:GC], ALU.mult, [gts[1].b, pps[1].b], [tm[1].b])
                            TT("pool", tm[0][:, :], tm[0][:, :], tm[1][:, :], ALU.add, [tm[0].b, tm[1].b], [tm[0].b])
                            TT("dve", tm[1][:, :], gts[2][:, :], pps[2][:, 0:GC], ALU.mult, [gts[2].b, pps[2].b], [tm[1].b])
                            TT("pool", mT[:, fc, :], tm[0][:, :], tm[1][:, :], ALU.add, [tm[0].b, tm[1].b], [mT.b])
                            yield
                        for q in range(NQ):
                            for half in range(2):
                                ps = PS[2 + half]
                                for k in range(KC):
                                    MM(ps[:, :], mT[:, k, q * 128:(q + 1) * 128], wo[:, k, half * 512:(half + 1) * 512], k == 0, k == KC - 1,
                                       [mT.b, wo.b], [ps.b])
                            ot = ots[oic[0] % 2]
                            oic[0] += 1
                            postnorm_residual(PS[2], PS[3], xt4s[g % 2][q], G1, ss2, ot, junk2)
                            DMA("sp", x1_d[s, t0 + q * 128:t0 + (q + 1) * 128, :], ot[:, :], [ot.b], [], ot.b)
                            yield

                    NGC = S // GC
                    interleave(c_norm(0), None)
                    for g in range(NGC):
                        interleave(c_main(g), c_norm(g + 1) if g + 1 < NGC else None)
                    pg.barrier()
                    pg.emit()
                if stop_after == "C":
                    continue
                with ExitStack() as st:
                    wu = sb("wu", [128, KC, 2 * DFF], BF16, st)
                    wd = sb("wd", [128, FC, D], BF16, st)
                    xts = [sb("xd%d" % i, [128, D], F32, st) for i in range(1)]
                    xrs = [sb("xr%d" % i, [128, D], F32, st) for i in range(1)]
                    xn = sb("xn", [128, D], F32, st)
                    junk = sb("junk", [128, D], BF16, st)
                    junk2 = sb("junk2", [128, D], BF16, st)
                    hT2s = [sb("hT2_%d" % j, [128, KC, GD], BF16, st) for j in range(2)]
                    actT = sb("actT", [128, FC, GD], BF16, st)
                    araw = [sb("araw%d" % i, [128, GD + 2], F32, st) for i in range(2)]
                    cv = [sb("cv%d" % i, [128, GD], F32, st) for i in range(2)]
                    ge = [sb("ge%d" % i, [128, GD], F32, st) for i in range(2)]
                    halo = sb("halo", [128, FC, 2], F32, st)
                    gsb = [sb("gs%d" % i, [128, GD], F32, st) for i in range(2)]
                    ots = [sb("ot%d" % i, [128, D], F32, st) for i in range(2)]
                    ss = sb("ss", [128, 8], F32, st)
                    ss2 = sb("ss2", [128, 8], F32, st)
                    usrc = w_up_d[l].rearrange("(k p) n -> p k n", p=128)
                    for c0 in range(0, 2 * DFF, 1408):
                        DMA("pool", wu[:, :, c0:c0 + 1408], usrc[:, :, c0:c0 + 1408], [], [wu.b], wu.b)
                    dsrc = w_down_d[l].rearrange("(k p) n -> p k n", p=128)
                    for k0 in range(0, FC, 11):
                        DMA("pool", wd[:, k0:k0 + 11, :], dsrc[:, k0:k0 + 11, :], [], [wd.b], wd.b)
                    MS("dve", halo[:, :, :], 0.0, [halo.b])
                    G2 = sb("G2", [128, D], F32, st)
                    DMA("sp", G2[:, :], G_d[l, s, 1], [], [G2.b], G2.b)
                    oid = [0]

                    def d_norm(g):
                        t0 = g * GD
                        hT2 = hT2s[g % 2]
                        for q in range(GD // 128):
                            xt = xts[0]
                            DMA("sp", xt[:, :], x1_d[s, t0 + q * 128:t0 + (q + 1) * 128, :], [], [xt.b], xt.b)
                            norm_hT(xt, xn, junk, ss, (lambda q: lambda k: (hT2[:, k, q * 128:(q + 1) * 128], hT2.b))(q), A2, B2, PS[0], PS[1])
                            yield

                    def d_main(g):
                        t0 = g * GD
                        hT2 = hT2s[g % 2]
                        for fc in range(FC):
                            pa_ = PS[2 + (fc % 2) * 2]
                            pgt = PS[3 + (fc % 2) * 2]
                            for k in range(KC):
                                MM(pa_[:, 0:GD], wu[:, k, fc * 128:(fc + 1) * 128], hT2[:, k, :], k == 0, k == KC - 1, [wu.b, hT2.b], [pa_.b])
                            for k in range(KC):
                                MM(pgt[:, 0:GD], wu[:, k, DFF + fc * 128:DFF + (fc + 1) * 128], hT2[:, k, :], k == 0, k == KC - 1,
                                   [wu.b, hT2.b], [pgt.b])
                            ar = araw[fc % 2]
                            c_ = cv[fc % 2]
                            g_ = ge[fc % 2]
                            gs_ = gsb[fc % 2]
                            CP("dve", gs_[:, :], pgt[:, 0:GD], [pgt.b], [gs_.b])
                            CP("pool", ar[:, 0:2], halo[:, fc, :], [halo.b], [ar.b])
                            CP("act", ar[:, 2:GD + 2], pa_[:, 0:GD], [pa_.b], [ar.b])
                            CP("pool", halo[:, fc, :], ar[:, GD:GD + 2], [ar.b], [halo.b])
                            ACT(c_[:, :], ar[:, 2:GD + 2], AF.Identity, [ar.b, small.b], [c_.b], bias=conv_bT[:, l, fc:fc + 1],
                                scale=conv_wT[:, l, 2, fc:fc + 1])
                            STT("dve", c_[:, :], ar[:, 1:GD + 1], conv_wT[:, l, 1, fc:fc + 1], c_[:, :], ALU.mult, ALU.add, [ar.b, small.b, c_.b], [c_.b])
                            STT("dve", c_[:, :], ar[:, 0:GD], conv_wT[:, l, 0, fc:fc + 1], c_[:, :], ALU.mult, ALU.add, [ar.b, small.b, c_.b], [c_.b])
                            ACT(g_[:, :], c_[:, :], AF.Gelu_apprx_tanh, [c_.b], [g_.b])
                            TT("pool", actT[:, fc, :], g_[:, :], gs_[:, :], ALU.mult, [g_.b, gs_.b], [actT.b])
                            yield
                        for q in range(GD // 128):
                            xr = xrs[0]
                            DMA("sp", xr[:, :], x1_d[s, t0 + q * 128:t0 + (q + 1) * 128, :], [], [xr.b], xr.b)
                            for half in range(2):
                                ps = PS[6 + half]
                                for k in range(FC):
                                    MM(ps[:, :], actT[:, k, q * 128:(q + 1) * 128], wd[:, k, half * 512:(half + 1) * 512], k == 0, k == FC - 1,
                                       [actT.b, wd.b], [ps.b])
                            ot = ots[oid[0] % 2]
                            oid[0] += 1
                            postnorm_residual(PS[6], PS[7], xr, G2, ss2, ot, junk2)
                            DMA("sp", xout_d[s, t0 + q * 128:t0 + (q + 1) * 128, :], ot[:, :], [ot.b], [], ot.b)
                            yield

                    NGD = S // GD
                    interleave(d_norm(0), None)
                    for g in range(NGD):
                        interleave(d_main(g), d_norm(g + 1) if g + 1 < NGD else None)
                    pg.barrier()
                    pg.emit()
    return nc


def _consts(S):
    NT = S // 128
    cst = np.zeros((128, NCST), np.float32)
    k = np.arange(128)[:, None]
    m = np.arange(128)[None, :]
    cst[:, 0:128] = (k == m)
    cst[:, 128:256] = (k <= m)
    cst[:, 256:384] = (k == 127)
    cst[:, 384:896] = np.tile((k == m).astype(np.float32), (1, 4))
    cst[:, 896:1408] = np.tile(np.where(k > m, NEGM, 0.0).astype(np.float32), (1, 4))
    cst[:, 1408:1408 + NIT] = (2.0 ** -(np.arange(NIT) + 1.0))[None, :]
    pos = np.arange(S, dtype=np.float32)
    inv_freq = (np.float32(10000.0) ** (-np.arange(0, 64, 2, dtype=np.float32) / np.float32(64))).astype(np.float32)
    ang = (pos[:, None] * inv_freq[None, :]).astype(np.float32)
    cos = np.cos(ang).astype(np.float32)
    sin = np.sin(ang).astype(np.float32)
    cs = np.concatenate([cos, sin, cos * np.float32(0.125), sin * np.float32(0.125)], axis=1).reshape(NT, 128, 128)
    ki = np.arange(128)[:, None, None]
    mm = np.arange(5)[None, :, None]
    qi = np.arange(128)[None, None, :]
    rel = 512 - 128 * mm + qi - ki
    qm = qi % 64
    valid = (rel >= qm - 63) & (rel <= qm + 512)
    relidx = np.clip(rel, -128, 128) + 128
    maskA = np.where(valid, 0.0, NEGM).astype(np.float32)
    maskA = np.broadcast_to(maskA[:, :, None, :], (128, 5, 4, 128)).reshape(128, 5 * 512)
    return cst, cs.astype(np.float32), relidx, np.ascontiguousarray(maskA)


def _prep(inputs, S, NSEQ, ncores):
    f = lambda a: np.ascontiguousarray(np.asarray(a, dtype=np.float32))
    cst, cs, relidx, maskA = _consts(S)
    x = f(inputs["x"])
    c = f(inputs["c"])
    b_ada = f(inputs["b_ada"])
    norm_g = f(inputs["norm_g"])
    rel_table = f(inputs["rel_table"])
    shared = {
        "w_ada": f(inputs["w_ada"]),
        "b_adaT": np.ascontiguousarray(b_ada.reshape(2, 48, 128).transpose(2, 0, 1)),
        "b_adaB": np.ascontiguousarray(np.broadcast_to(
            b_ada.reshape(2, 6, D)[:, [2, 5], :][None], (128, 2, 2, D))),
        "normgT": np.ascontiguousarray(norm_g.reshape(2, 4, KC, 128).transpose(3, 0, 1, 2)),
        "normgB": np.ascontiguousarray(np.broadcast_to(norm_g[:, [1, 3], :][None], (128, 2, 2, D))),
        "w_in": f(inputs["w_in"]),
        "b_gateT": np.ascontiguousarray(f(inputs["b_gate"]).reshape(2, 3, KC, 128).transpose(3, 0, 1, 2)),
        "biasT": np.ascontiguousarray(rel_table[:, :, relidx].transpose(0, 2, 3, 1, 4).reshape(2, 128, 5 * 512)),
        "maskA": maskA,
        "b_forgetB": np.ascontiguousarray(np.broadcast_to(f(inputs["b_forget"])[None], (128, 2, 4))),
        "w_branch": f(inputs["w_branch"]),
        "w_out": f(inputs["w_out"]),
        "w_up": f(inputs["w_up"]),
        "conv_wT": np.ascontiguousarray(f(inputs["conv_w"]).reshape(2, 3, FC, 128).transpose(3, 0, 1, 2)),
        "conv_bT": np.ascontiguousarray(f(inputs["conv_b"]).reshape(2, FC, 128).transpose(2, 0, 1)),
        "w_down": f(inputs["w_down"]),
        "cs": cs,
        "cst": cst,
    }
    maps = []
    for i in range(ncores):
        d = dict(shared)
        d["x"] = np.ascontiguousarray(x[i * NSEQ:(i + 1) * NSEQ])
        cc = c[i * NSEQ:(i + 1) * NSEQ]
        d["cT"] = np.ascontiguousarray(cc.reshape(NSEQ, KC, 128).transpose(2, 1, 0))
        maps.append(d)
    return maps


def run(inputs, cfg, ncores=8):
    nc = build(cfg)
    maps = _prep(inputs, cfg["S"], cfg["NSEQ"], ncores)
    res = run_bass_kernel_spmd(nc, maps, core_ids=list(range(ncores)))
    return res


def kernel(**inputs):
    x = np.asarray(inputs["x"])
    B, S, _ = x.shape
    ncores = 8
    NSEQ = B // ncores
    cfg = dict(S=S, NSEQ=NSEQ, NL=2)
    res = run(inputs, cfg, ncores)
    out = np.concatenate([np.asarray(r["out"]) for r in res.results], axis=0)
    return out.astype(np.float32)
```

```python
import numpy as np
from contextlib import ExitStack
import concourse.bass as bass
import concourse.mybir as mybir
from concourse.bass_utils import run_bass_kernel_spmd

F32 = mybir.dt.float32
BF16 = mybir.dt.bfloat16
AF = mybir.ActivationFunctionType
ALU = mybir.AluOpType
AX = mybir.AxisListType

D = 1024
KC = 8
NIN = 5320
DFF = 2816
FC = 22
NEGM = -30000.0
NIT = 12
EPS = 1e-6
NCST = 128 * 3 + 512 + 512 + NIT

OFF = dict(qa=0, ka=256, va=512, qb=768, kb=1024, vb=1280, fb=1536, qc=1540, kc=1796, vc=1860,
           qi=1924, ki=2180, wi=2244, zg=2248)
DST = dict(qa=0, ka=256, qb=512, kb=768, va=1024, vb=1280, qc=1536, qi=1792,
           kc=2048, ki=2112, vc=2176, fb=2240, wi=2244)
WID = dict(qa=256, ka=256, qb=256, kb=256, va=256, vb=256, qc=256, qi=256, kc=64, ki=64, vc=64, fb=4, wi=4)
NQKV = 2248


class Buf:
    __slots__ = ("name", "w", "r", "rd", "sem", "cnt", "excl")

    def __init__(self, name=""):
        self.name = name
        self.excl = False
        self.w = None
        self.r = {}
        self.rd = []
        self.sem = None
        self.cnt = 0


class Prog:
    ENG = ("pe", "act", "dve", "pool", "sp")

    def __init__(self, nc, stack):
        self.nc = nc
        self.stack = stack
        self.ops = []
        self.emitted = 0
        self.esem = {e: stack.enter_context(nc.semaphore("es_" + e)) for e in ("pe", "act", "dve", "pool")}
        self.ecnt = {e: 0 for e in self.esem}
        self.seen = {e: {} for e in self.ENG}
        self.tok = Buf("tok")
        self.nsem = 0
        self.last_bar = None
        self.free_sems = []
        self.sem_bufs = []

    def dsem(self, buf, fresh=False):
        if buf.sem is None:
            if fresh:
                self.nsem += 1
                buf.sem = self.stack.enter_context(self.nc.semaphore("dq%d" % self.nsem))
                buf.cnt = 0
                return buf.sem
            if self.free_sems:
                buf.sem, buf.cnt = self.free_sems.pop()
            else:
                self.nsem += 1
                buf.sem = self.stack.enter_context(self.nc.semaphore("ds%d" % self.nsem))
                buf.cnt = 0
            self.sem_bufs.append(buf)
        return buf.sem

    def release_sems(self):
        for b in self.sem_bufs:
            self.free_sems.append((b.sem, b.cnt))
            b.sem = None
            for e in self.ENG:
                self.seen[e].pop(id(b), None)
        self.sem_bufs = []

    def op(self, eng, fn, r=(), w=(), dma=None, tok=True):
        deps = set()
        w = list(w) + [b for b in r if b.excl and b not in w]
        r = [b for b in r if not b.excl]
        if tok:
            r.append(self.tok)
        for b in r:
            if b.w is not None:
                deps.add(b.w)
        for b in w:
            if b.w is not None:
                wo = self.ops[b.w]
                if not (dma is not None and wo["dma"] is dma and wo["eng"] == eng):
                    deps.add(b.w)
            deps.update(b.r.values())
            deps.update(b.rd)
        if tok and self.last_bar is not None:
            deps = {d for d in deps if d >= self.last_bar}
        i = len(self.ops)
        o = dict(eng=eng, fn=fn, deps=deps, dma=dma, needed=False, waits=None, cnt=None, dval=None)
        if dma is not None:
            self.dsem(dma, fresh=(eng == "pool"))
            dma.cnt += 16
            o["dval"] = dma.cnt
        self.ops.append(o)
        for b in r:
            if dma is not None:
                b.rd.append(i)
            else:
                b.r[eng] = i
        for b in w:
            b.w = i
            b.r = {}
            b.rd = []
        return i

    def barrier(self):
        self.last_bar = self.op("dve", lambda e: e.memset(self.bar_tile, 0.0), w=[self.tok], tok=False)
        self.ops[self.last_bar]["needed"] = True

    def emit(self):
        nc = self.nc
        ops = self.ops[self.emitted:]
        for o in ops:
            eng = o["eng"]
            seen = self.seen[eng]
            per = {}
            dmaw = {}
            for d in o["deps"]:
                do = self.ops[d]
                if do["dma"] is not None:
                    key = id(do["dma"])
                    if do["dval"] > seen.get(key, 0):
                        if key not in dmaw or dmaw[key][1] < do["dval"]:
                            dmaw[key] = (do["dma"], do["dval"])
                else:
                    f = do["eng"]
                    if f == "pe" and eng == "pe":
                        continue
                    if d > seen.get(f, -1):
                        per[f] = max(per.get(f, -1), d)
            w = []
            for f, d in per.items():
                seen[f] = d
                self.ops[d]["needed"] = True
                w.append(("e", f, d))
            for key, (buf, val) in dmaw.items():
                seen[key] = val
                w.append(("d", buf, val))
            o["waits"] = w
        for o in ops:
            if o["dma"] is None and o["needed"]:
                self.ecnt[o["eng"]] += 1
                o["cnt"] = self.ecnt[o["eng"]]
        per_eng = {e: [] for e in self.ENG}
        for o in ops:
            per_eng[o["eng"]].append(o)

        def run(eng_name):
            def body(e):
                for o in per_eng[eng_name]:
                    for wt in o["waits"]:
                        if wt[0] == "e":
                            e.wait_ge(self.esem[wt[1]], self.ops[wt[2]]["cnt"])
                        else:
                            e.wait_ge(wt[1].sem, wt[2])
                    if o["fn"] is None:
                        continue
                    ins = o["fn"](e)
                    if o["dma"] is not None:
                        ins.then_inc(o["dma"].sem, 16)
                    elif o["needed"]:
                        ins.then_inc(self.esem[eng_name], 1)
            return body

        with nc.Block() as block:
            block.tensor(run("pe"))
            block.scalar(run("act"))
            block.vector(run("dve"))
            block.gpsimd(run("pool"))
            block.sync(run("sp"))
        self.emitted = len(self.ops)
        if self.last_bar == len(self.ops) - 1:
            self.release_sems()


class T:
    def __init__(self, ap, name=""):
        self.ap = ap
        self.b = Buf(name)

    def __getitem__(self, k):
        return self.ap[k]


def build(cfg):
    S = cfg["S"]
    NSEQ = cfg["NSEQ"]
    NL = cfg["NL"]
    NT = S // 128
    GD = 256
    TOPK = min(256, S // 4)
    stop_after = cfg.get("stop_after", "D")
    dbg = cfg.get("dbg", False)
    skip = cfg.get("skip", "")

    nc = bass.Bass("TRN2", target_bir_lowering=False)

    def din(name, shape, dt=F32):
        return nc.dram_tensor(name, list(shape), dt, kind="ExternalInput").ap()

    x_d = din("x", [NSEQ, S, D])
    cT_d = din("cT", [128, KC, NSEQ])
    w_ada_d = din("w_ada", [2, D, 6 * D])
    b_adaT_d = din("b_adaT", [128, 2, 48])
    b_adaB_d = din("b_adaB", [128, 2, 2, D])
    normgT_d = din("normgT", [128, 2, 4, KC])
    normgB_d = din("normgB", [128, 2, 2, D])
    w_in_d = din("w_in", [2, D, NIN])
    b_gateT_d = din("b_gateT", [128, 2, 3, KC])
    biasT_d = din("biasT", [2, 128, 5 * 512])
    maskA_d = din("maskA", [128, 5 * 512])
    b_forgetB_d = din("b_forgetB", [128, 2, 4])
    w_branch_d = din("w_branch", [2, 3, 256, D])
    w_out_d = din("w_out", [2, D, D])
    w_up_d = din("w_up", [2, D, 2 * DFF])
    conv_wT_d = din("conv_wT", [128, 2, 3, FC])
    conv_bT_d = din("conv_bT", [128, 2, FC])
    w_down_d = din("w_down", [2, DFF, D])
    cs_d = din("cs", [NT, 128, 128])
    cst_d = din("cst", [128, NCST])
    out_d = nc.dram_tensor("out", [NSEQ, S, D], F32, kind="ExternalOutput").ap()
    xs_d = [nc.dram_tensor("xs%d" % i, [NSEQ, S, D], F32, kind="Internal").ap() for i in range(2)]
    yT_d = nc.dram_tensor("yT", [NSEQ, 128, 6, S], BF16, kind="ExternalOutput" if dbg else "Internal").ap()

    stack = ExitStack()
    with stack:
        pg = Prog(nc, stack)

        uid = [0]

        def sb(name, shape, dt, st=stack):
            uid[0] += 1
            return T(st.enter_context(nc.sbuf_tensor("%s_%d" % (name, uid[0]), list(shape), dt)), name)

        def MM(out, lhsT, rhs, start, stop, r, w):
            pg.op("pe", lambda e: e.matmul(out=out, lhsT=lhsT, rhs=rhs, start=start, stop=stop,
                                           skip_group_check=True), r=r, w=w)

        def TR(out, in_, ident, r, w):
            pg.op("pe", lambda e: e.transpose(out=out, in_=in_, identity=ident), r=r, w=w)

        def ACT(out, in_, func, r, w, bias=None, scale=None, accum_out=None):
            kw = {}
            if bias is not None:
                kw["bias"] = bias
            if scale is not None:
                kw["scale"] = scale
            if accum_out is not None:
                kw["accum_out"] = accum_out
            pg.op("act", lambda e: e.activation(out=out, in_=in_, func=func, **kw), r=r, w=w)

        def TS(eng, out, in0, s1, s2, op0, op1, r, w, accum_out=None):
            kw = {}
            if op1 is not None:
                kw["op1"] = op1
            if accum_out is not None:
                kw["accum_out"] = accum_out
            pg.op(eng, lambda e: e.tensor_scalar(out=out, in0=in0, scalar1=s1, scalar2=s2, op0=op0, **kw), r=r, w=w)

        def TT(eng, out, in0, in1, op, r, w):
            pg.op(eng, lambda e: e.tensor_tensor(out=out, in0=in0, in1=in1, op=op), r=r, w=w)

        def STT(eng, out, in0, scalar, in1, op0, op1, r, w):
            pg.op(eng, lambda e: e.scalar_tensor_tensor(out=out, in0=in0, scalar=scalar, in1=in1, op0=op0, op1=op1), r=r, w=w)

        def CP(eng, out, in_, r, w):
            if eng == "act":
                pg.op("act", lambda e: e.copy(out=out, in_=in_), r=r, w=w)
            else:
                pg.op(eng, lambda e: e.tensor_copy(out=out, in_=in_), r=r, w=w)

        def MS(eng, ap, val, w):
            pg.op(eng, lambda e: e.memset(ap, val), w=w)

        def DMA(eng, out, in_, r, w, sem):
            pg.op(eng, lambda e: e.dma_start(out=out, in_=in_), r=r, w=w, dma=sem)

        bar = sb("bar", [128, 8], F32)
        pg.bar_tile = bar[:, 0:1]
        PS = [T(stack.enter_context(nc.psum_tensor("ps%d" % i, [128, 512], F32)), "ps%d" % i) for i in range(8)]
        PSB = [p[:, :].bitcast(BF16) for p in PS]
        for p in PS:
            p.b.excl = True

        cst = sb("cst", [128, NCST], F32)
        identF = cst[:, 0:128]
        triF = cst[:, 128:256]
        sel127 = cst[:, 256:384]
        i4F = cst[:, 384:896]
        tribF = cst[:, 896:1408]
        pow2 = cst[:, 1408:1408 + NIT]
        identB = sb("identB", [128, 128], BF16)
        i4B = sb("i4B", [128, 512], BF16)
        tribB = sb("tribB", [128, 512], BF16)
        DMA("sp", cst[:, :], cst_d[:, :], [], [cst.b], cst.b)
        CP("dve", identB[:, :], identF, [cst.b], [identB.b])
        CP("dve", i4B[:, :], i4F, [cst.b], [i4B.b])
        CP("dve", tribB[:, :], tribF, [cst.b], [tribB.b])

        NSM = 96 + 64 + 48 + 8 + 6 * FC + 2 * FC + KC * NSEQ
        small = sb("small", [128, NSM], F32)
        o = 0
        b_adaT = small[:, o:o + 96].rearrange("p (l c) -> p l c", l=2); o += 96
        normgT = small[:, o:o + 64].rearrange("p (l j k) -> p l j k", l=2, j=4); o += 64
        b_gateT = small[:, o:o + 48].rearrange("p (l j k) -> p l j k", l=2, j=3); o += 48
        b_forgetB = small[:, o:o + 8].rearrange("p (l h) -> p l h", l=2); o += 8
        conv_wT = small[:, o:o + 6 * FC].rearrange("p (l j f) -> p l j f", l=2, j=3); o += 6 * FC
        conv_bT = small[:, o:o + 2 * FC].rearrange("p (l f) -> p l f", l=2); o += 2 * FC
        cT = small[:, o:o + KC * NSEQ].rearrange("p (k b) -> p k b", k=KC); o += KC * NSEQ
        for dst, src in ((b_adaT, b_adaT_d), (normgT, normgT_d), (b_gateT, b_gateT_d), (b_forgetB, b_forgetB_d),
                         (conv_wT, conv_wT_d), (conv_bT, conv_bT_d), (cT, cT_d)):
            DMA("sp", dst, src, [], [small.b], small.b)
        cact = sb("cact", [128, KC, NSEQ], F32)
        csig = sb("csig", [128, KC, NSEQ], F32)
        ACT(csig[:, :, :], cT, AF.Sigmoid, [small.b], [csig.b])
        TT("dve", cact[:, :, :], cT, csig[:, :, :], ALU.mult, [small.b, csig.b], [cact.b])
        modT = sb("modT", [128, NL, NSEQ, 4, KC], F32)
        G_d = nc.dram_tensor("Gscr", [NL, NSEQ, 2, 128, D], F32, kind="Internal").ap()
        biasAll = sb("biasAll", [128, NL, 5 * 512], BF16)

        with ExitStack() as st0:
            wad = [sb("wad%d" % i, [128, KC, 1024], F32, st0) for i in range(2)]
            cactB = sb("cactB", [128, NSEQ, KC, 128], F32, st0)
            gtl = [sb("gtl%d" % i, [128, D], F32, st0) for i in range(2)]
            biasF = sb("biasF", [128, 5 * 512], F32, st0)
            maskF = sb("maskF", [128, 5 * 512], F32, st0)
            DMA("sp", maskF[:, :], maskA_d[:, :], [], [maskF.b], maskF.b)
            for l in range(NL):
                DMA("sp", biasF[:, :], biasT_d[l], [], [biasF.b], biasF.b)
                TT("dve", biasAll[:, l, :], biasF[:, :], maskF[:, :], ALU.add, [biasF.b, maskF.b], [biasAll.b])
            for s in range(NSEQ):
                CP("dve", cactB[:, s, :, :], cact[:, :, s:s + 1].to_broadcast([128, KC, 128]), [cact.b], [cactB.b])
            gi = 0
            badaB = sb("badaB", [128, D], F32, st0)
            gB = sb("gB", [128, D], F32, st0)
            mtmp = sb("mtmp", [128, NSEQ], F32, st0)
            for l in range(NL):
                for sec in range(6):
                    wt = wad[(l * 6 + sec) % 2]
                    src = w_ada_d[l].rearrange("(k p) n -> p k n", p=128)[:, :, sec * 1024:(sec + 1) * 1024]
                    for k0 in range(0, KC, 2):
                        DMA("sp", wt[:, k0:k0 + 2, :], src[:, k0:k0 + 2, :], [], [wt.b], wt.b)
                    if sec in (2, 5):
                        j = 0 if sec == 2 else 1
                        DMA("sp", badaB[:, :], b_adaB_d[:, l, j, :], [], [badaB.b], badaB.b)
                        DMA("sp", gB[:, :], normgB_d[:, l, j, :], [], [gB.b], gB.b)
                        for s in range(NSEQ):
                            g = gtl[gi % 2]
                            gi += 1
                            for half in range(2):
                                ps = PS[half]
                                for k in range(KC):
                                    MM(ps[:, :], cactB[:, s, k, :], wt[:, k, half * 512:(half + 1) * 512],
                                       k == 0, k == KC - 1, [cactB.b, wt.b], [ps.b])
                                hs = slice(half * 512, (half + 1) * 512)
                                TT("dve", g[:, hs], ps[:, :], badaB[:, hs], ALU.add, [ps.b, badaB.b], [g.b])
                                TT("dve", g[:, hs], g[:, hs], gB[:, hs], ALU.mult, [g.b, gB.b], [g.b])
                            DMA("sp", G_d[l, s, j], g[:, :], [g.b], [], g.b)
                    else:
                        jj = {0: 1, 1: 0, 3: 3, 4: 2}[sec]
                        for kc in range(KC):
                            ps = PS[2 + kc % 2]
                            for k in range(KC):
                                MM(ps[:, 0:NSEQ], wt[:, k, kc * 128:(kc + 1) * 128], cact[:, k, :],
                                   k == 0, k == KC - 1, [cact.b, wt.b], [ps.b])
                            cc = sec * 8 + kc
                            if sec in (0, 3):
                                TS("dve", modT[:, l, :, jj, kc], ps[:, 0:NSEQ], b_adaT[:, l, cc:cc + 1], None, ALU.add, None,
                                   [ps.b, small.b], [modT.b])
                            else:
                                ng = 0 if sec == 1 else 2
                                TS("dve", mtmp[:, :], ps[:, 0:NSEQ], b_adaT[:, l, cc:cc + 1], 1.0, ALU.add, ALU.add,
                                   [ps.b, small.b], [mtmp.b])
                                TS("dve", modT[:, l, :, jj, kc], mtmp[:, :], normgT[:, l, ng, kc:kc + 1], None, ALU.mult, None,
                                   [mtmp.b, small.b], [modT.b])
            pg.barrier()
            pg.emit()

        def norm_hT(xt, xn, junk, ss, hT_ap_fn, A, Bv, psA, psB, extra_r=()):
            ACT(junk[:, 0:D], xt[:, :], AF.Square, [xt.b], [junk.b, ss.b], accum_out=ss[:, 0:1])
            ACT(ss[:, 1:2], ss[:, 0:1], AF.Sqrt, [ss.b], [ss.b], bias=EPS, scale=1.0 / D)
            pg.op("dve", lambda e: e.reciprocal(out=ss[:, 2:3], in_=ss[:, 1:2]), r=[ss.b], w=[ss.b])
            ACT(xn[:, :], xt[:, :], AF.Copy, [xt.b, ss.b], [xn.b], scale=ss[:, 2:3])
            for half, ps in ((0, psA), (1, psB)):
                for kk in range(4):
                    k = half * 4 + kk
                    TR(ps[:, kk * 128:(kk + 1) * 128], xn[:, k * 128:(k + 1) * 128], identF, [xn.b, cst.b], [ps.b])
                for kk in range(4):
                    k = half * 4 + kk
                    outap, wb = hT_ap_fn(k)
                    if kk % 2 == 0:
                        ACT(outap, ps[:, kk * 128:(kk + 1) * 128], AF.Identity, [ps.b, modT.b], [wb],
                            bias=Bv[:, k:k + 1], scale=A[:, k:k + 1])
                    else:
                        TS("dve", outap, ps[:, kk * 128:(kk + 1) * 128], A[:, k:k + 1], Bv[:, k:k + 1], ALU.mult, ALU.add,
                           [ps.b, modT.b], [wb])

        def postnorm_residual(psA, psB, xres, G, ss, ot, junk):
            ACT(junk[:, 0:512], psA[:, :], AF.Square, [psA.b], [junk.b, ss.b], accum_out=ss[:, 4:5])
            ACT(junk[:, 512:1024], psB[:, :], AF.Square, [psB.b], [junk.b, ss.b], accum_out=ss[:, 5:6])
            TT("dve", ss[:, 6:7], ss[:, 4:5], ss[:, 5:6], ALU.add, [ss.b], [ss.b])
            ACT(ss[:, 3:4], ss[:, 6:7], AF.Sqrt, [ss.b], [ss.b], bias=EPS, scale=1.0 / D)
            pg.op("dve", lambda e: e.reciprocal(out=ss[:, 7:8], in_=ss[:, 3:4]), r=[ss.b], w=[ss.b])
            for half, ps in ((0, psA), (1, psB)):
                hs = slice(half * 512, (half + 1) * 512)
                STT("dve", ot[:, hs], ps[:, :], ss[:, 7:8], G[:, hs], ALU.mult, ALU.mult, [ps.b, ss.b, G.b], [ot.b])
                TT("pool", ot[:, hs], ot[:, hs], xres[:, hs], ALU.add, [ot.b, xres.b], [ot.b])

        def interleave(g1, g2):
            d1 = g1 is None
            d2 = g2 is None
            while not (d1 and d2):
                if not d1:
                    try:
                        next(g1)
                    except StopIteration:
                        d1 = True
                if not d2:
                    try:
                        next(g2)
                    except StopIteration:
                        d2 = True

        for l in range(NL if stop_after != "0" else 0):
            xin_d = x_d if l == 0 else xs_d[1]
            x1_d = xs_d[0]
            xout_d = out_d if l == NL - 1 else xs_d[1]
            for s in range(NSEQ):
                A1 = modT[:, l, s, 0, :]
                B1 = modT[:, l, s, 1, :]
                A2 = modT[:, l, s, 2, :]
                B2 = modT[:, l, s, 3, :]
                with ExitStack() as st:
                    wq = sb("wq", [128, KC, NQKV], BF16, st)
                    KaT = sb("KaT", [64, 4, 1024], BF16, st)
                    Va = sb("Va", [128, 8, 4, 65], BF16, st)
                    KbT = sb("KbT", [70, 4, S], BF16, st)
                    Vb = sb("Vb", [128, NT, 4, 65], BF16, st)
                    KcT = sb("KcT", [64, S], BF16, st)
                    KiT = sb("KiT", [64, S], BF16, st)
                    Vc = sb("Vc", [128, NT, 65], BF16, st)
                    kcb = [Buf("kc%d" % i) for i in range(NT)]
                    kib = [Buf("ki%d" % i) for i in range(NT)]
                    vcb = [Buf("vc%d" % i) for i in range(NT)]
                    xts = [sb("xt%d" % i, [128, D], F32, st) for i in range(2)]
                    xn = sb("xn", [128, D], F32, st)
                    hT = sb("hT", [128, KC, 128], BF16, st)
                    css = [sb("cs%d" % i, [128, 128], F32, st) for i in range(2)]
                    zqa = sb("zqa", [128, 256], BF16, st)
                    zka = sb("zka", [128, 256], BF16, st)
                    zqb = sb("zqb", [128, 4, 70], BF16, st)
                    zkb = sb("zkb", [128, 4, 70], BF16, st)
                    zrq = sb("zrq", [128, 8, 64], BF16, st)
                    zrk = sb("zrk", [128, 2, 64], BF16, st)
                    rt1 = sb("rt1", [128, 8, 32], F32, st)
                    rt2 = sb("rt2", [128, 8, 32], F32, st)
                    QaT = sb("QaT", [64, 4, 128], BF16, st)
                    QbT = sb("QbT", [70, 4, 128], BF16, st)
                    QcTs = [sb("QcT%d" % i, [64, 4, 128], BF16, st) for i in range(2)]
                    QiTs = [sb("QiT%d" % i, [64, 4, 128], BF16, st) for i in range(2)]
                    fos = [sb("fo%d" % i, [128, 64], F32, st) for i in range(2)]
                    fob = sb("fob", [128, 16], BF16, st)
                    cum = [sb("cum%d" % i, [128, 4], F32, st) for i in range(2)]
                    ss = sb("ss", [128, 8], F32, st)
                    bis = sb("bis", [128, 8 + NIT], F32, st)
                    score = sb("score", [128, S], F32, st)
                    MBt = sb("MBt", [128, S], BF16, st)
                    junk = sb("junk", [128, D], BF16, st)
                    rts = [sb("rts%d" % i, [128, 512], F32, st) for i in range(2)]
                    pTs1 = [sb("pTa%d" % i, [128, 512], BF16, st) for i in range(5)]
                    pTs2 = [sb("pTb%d" % i, [128, 512], BF16, st) for i in range(3)]
                    yts = [sb("yt%d" % i, [128, 768], BF16, st) for i in range(2)]
                    rinv1 = sb("rinv1", [128, 4], F32, st)
                    rinv2 = sb("rinv2", [128, 4], F32, st)
                    yTs = sb("yTs", [128, 6, 128], BF16, st)

                    wsrc = w_in_d[l].rearrange("(k p) n -> p k n", p=128)
                    for nm in ("qa", "ka", "qb", "kb", "va", "vb", "qc", "qi", "kc", "ki", "vc", "fb", "wi"):
                        DMA("pool", wq[:, :, DST[nm]:DST[nm] + WID[nm]], wsrc[:, :, OFF[nm]:OFF[nm] + WID[nm]], [], [wq.b], wq.b)
                    MS("dve", Va[:, :, :, 64:65], 1.0, [Va.b])
                    MS("dve", Vb[:, :, :, 64:65], 1.0, [Vb.b])
                    MS("dve", Vc[:, :, 64:65], 1.0, vcb)
                    MS("dve", zqb[:, :, 67:70], 1.0, [zqb.b])
                    MS("dve", zkb[:, :, 64:67], 1.0, [zkb.b])
                    MS("dve", cum[1][:, :], 0.0, [cum[1].b])

                    cnt1 = [0, 0]
                    cnt2 = [0, 0]

                    def recip_norm(acc, rinv, ydst):
                        accv = acc[:, 0:260].rearrange("p (h d) -> p h d", h=4)
                        pg.op("dve", lambda e: e.reciprocal(out=rinv[:, :], in_=accv[:, :, 64]), r=[acc.b], w=[rinv.b])
                        return accv

                    def stage1(t):
                        xt = xts[t % 2]
                        cs = css[t % 2]
                        fo = fos[t % 2]
                        QcT = QcTs[t % 2]
                        QiT = QiTs[t % 2]
                        yt = yts[t % 2]
                        DMA("sp", xt[:, :], xin_d[s, t * 128:(t + 1) * 128, :], [], [xt.b], xt.b)
                        DMA("sp", cs[:, :], cs_d[t], [], [cs.b], cs.b)
                        norm_hT(xt, xn, junk, ss, lambda k: (hT[:, k, :], hT.b), A1, B1, PS[0], PS[7])
                        yield
                        cosk = cs[:, 0:32]
                        sink = cs[:, 32:64]
                        cosq = cs[:, 64:96]
                        sinq = cs[:, 96:128]
                        blocks = [(0, 512), (512, 512), (1024, 512), (1536, 512), (2048, 200)]
                        for bi, (c0, cw) in enumerate(blocks):
                            ps = PS[0] if bi % 2 == 0 else PS[7]
                            for k in range(KC):
                                MM(ps[:, 0:cw], hT[:, k, :], wq[:, k, c0:c0 + cw], k == 0, k == KC - 1, [hT.b, wq.b], [ps.b])
                            if bi == 0:
                                TS("dve", zqa[:, :], ps[:, 0:256], 0.125, None, ALU.mult, None, [ps.b], [zqa.b])
                                CP("act", zka[:, :], ps[:, 256:512], [ps.b], [zka.b])
                            elif bi == 1:
                                TS("dve", zqb[:, :, 0:64], ps[:, 0:256].rearrange("p (h d) -> p h d", h=4), 0.125, None,
                                   ALU.mult, None, [ps.b], [zqb.b])
                                CP("act", zkb[:, :, 0:64], ps[:, 256:512].rearrange("p (h d) -> p h d", h=4), [ps.b], [zkb.b])
                            elif bi == 2:
                                CP("act", Va[:, t % 8, :, 0:64], ps[:, 0:256].rearrange("p (h d) -> p h d", h=4), [ps.b], [Va.b])
                                CP("dve", Vb[:, t, :, 0:64], ps[:, 256:512].rearrange("p (h d) -> p h d", h=4), [ps.b], [Vb.b])
                            elif bi == 3:
                                z3 = ps[:, 0:512].rearrange("p (h d) -> p h d", h=8)
                                x1 = z3[:, :, 0:32]
                                x2 = z3[:, :, 32:64]
                                cb = cosq.unsqueeze(1).to_broadcast([128, 8, 32])
                                sbb = sinq.unsqueeze(1).to_broadcast([128, 8, 32])
                                TT("dve", rt1[:, :, :], x1, cb, ALU.mult, [ps.b, cs.b], [rt1.b])
                                TT("dve", rt2[:, :, :], x2, sbb, ALU.mult, [ps.b, cs.b], [rt2.b])
                                TT("pool", zrq[:, :, 0:32], rt1[:, :, :], rt2[:, :, :], ALU.subtract, [rt1.b, rt2.b], [zrq.b])
                                TT("dve", rt1[:, :, :], x1, sbb, ALU.mult, [ps.b, cs.b], [rt1.b])
                                TT("dve", rt2[:, :, :], x2, cb, ALU.mult, [ps.b, cs.b], [rt2.b])
                                TT("pool", zrq[:, :, 32:64], rt1[:, :, :], rt2[:, :, :], ALU.add, [rt1.b, rt2.b], [zrq.b])
                            else:
                                z4 = ps[:, 0:128].rearrange("p (h d) -> p h d", h=2)
                                x1 = z4[:, :, 0:32]
                                x2 = z4[:, :, 32:64]
                                cb = cosk.unsqueeze(1).to_broadcast([128, 2, 32])
                                sbb = sink.unsqueeze(1).to_broadcast([128, 2, 32])
                                TT("dve", rt1[:, 0:2, :], x1, cb, ALU.mult, [ps.b, cs.b], [rt1.b])
                                TT("dve", rt2[:, 0:2, :], x2, sbb, ALU.mult, [ps.b, cs.b], [rt2.b])
                                TT("pool", zrk[:, :, 0:32], rt1[:, 0:2, :], rt2[:, 0:2, :], ALU.subtract, [rt1.b, rt2.b], [zrk.b])
                                TT("dve", rt1[:, 0:2, :], x1, sbb, ALU.mult, [ps.b, cs.b], [rt1.b])
                                TT("dve", rt2[:, 0:2, :], x2, cb, ALU.mult, [ps.b, cs.b], [rt2.b])
                                TT("pool", zrk[:, :, 32:64], rt1[:, 0:2, :], rt2[:, 0:2, :], ALU.add, [rt1.b, rt2.b], [zrk.b])
                                CP("act", Vc[:, t, 0:64], ps[:, 128:192], [ps.b], [vcb[t]])
                                TT("dve", fo[:, 0:4], ps[:, 192:196], b_forgetB[:, l, :], ALU.add, [ps.b, small.b], [fo.b])
                                ACT(fo[:, 16:20], ps[:, 196:200], AF.Abs, [ps.b], [fo.b], scale=0.5)
                                TS("dve", fo[:, 20:24], ps[:, 196:200], 0.0, 2.0, ALU.is_ge, ALU.mult, [ps.b], [fo.b])
                                TS("dve", fo[:, 20:24], fo[:, 20:24], -1.0, None, ALU.add, None, [fo.b], [fo.b])
                                ACT(fo[:, 4:8], fo[:, 0:4], AF.Exp, [fo.b], [fo.b], scale=-1.0)
                                ACT(fo[:, 8:12], fo[:, 4:8], AF.Ln, [fo.b], [fo.b], bias=1.0)
                            yield
                        cprev = cum[(t + 1) % 2]
                        ccur = cum[t % 2]
                        psc = PS[6]
                        MM(psc[:, 0:4], triF, fo[:, 8:12], True, False, [cst.b, fo.b], [psc.b])
                        MM(psc[:, 0:4], sel127, cprev[:, :], False, True, [cst.b, cprev.b], [psc.b])
                        CP("dve", ccur[:, :], psc[:, 0:4], [psc.b], [ccur.b])
                        CP("dve", fob[:, 0:4], ccur[:, :], [ccur.b], [fob.b])
                        TT("dve", fo[:, 24:28], ccur[:, :], fob[:, 0:4], ALU.subtract, [ccur.b, fob.b], [fo.b])
                        CP("dve", fob[:, 4:8], fo[:, 24:28], [fo.b], [fob.b])
                        TT("dve", fo[:, 28:32], fo[:, 24:28], fob[:, 4:8], ALU.subtract, [fo.b, fob.b], [fo.b])
                        CP("dve", fob[:, 8:12], fo[:, 28:32], [fo.b], [fob.b])
                        for i3 in range(3):
                            src3 = fob[:, 4 * i3:4 * i3 + 4].unsqueeze(2)
                            CP("dve", zkb[:, :, 67 + i3:68 + i3], src3, [fob.b], [zkb.b])
                            TS("dve", zqb[:, :, 64 + i3:65 + i3], src3, -1.0, None, ALU.mult, None, [fob.b], [zqb.b])
                        yield
                        pst = PSB[7]
                        pstb = PS[7].b
                        for h in range(4):
                            TR(pst[0:64, h * 128:(h + 1) * 128], zqa[:, h * 64:(h + 1) * 64], identB[:, :], [zqa.b, identB.b], [pstb])
                            TR(pst[0:64, 512 + h * 128:512 + (h + 1) * 128], zka[:, h * 64:(h + 1) * 64], identB[:, :], [zka.b, identB.b], [pstb])
                        CP("dve", QaT[:, :, :], pst[0:64, 0:512].rearrange("p (h q) -> p h q", h=4), [pstb], [QaT.b])
                        CP("act", KaT[:, :, (t % 8) * 128:(t % 8 + 1) * 128], pst[0:64, 512:1024].rearrange("p (h q) -> p h q", h=4), [pstb], [KaT.b])
                        yield
                        for h in range(4):
                            TR(pst[0:70, h * 128:(h + 1) * 128], zqb[:, h, :], identB[:, :], [zqb.b, identB.b], [pstb])
                            TR(pst[0:70, 512 + h * 128:512 + (h + 1) * 128], zkb[:, h, :], identB[:, :], [zkb.b, identB.b], [pstb])
                        CP("dve", QbT[:, :, :], pst[0:70, 0:512].rearrange("p (h q) -> p h q", h=4), [pstb], [QbT.b])
                        CP("act", KbT[:, :, t * 128:(t + 1) * 128], pst[0:70, 512:1024].rearrange("p (h q) -> p h q", h=4), [pstb], [KbT.b])
                        yield
                        for h in range(4):
                            TR(pst[0:64, h * 128:(h + 1) * 128], zrq[:, h, :], identB[:, :], [zrq.b, identB.b], [pstb])
                            TR(pst[0:64, 512 + h * 128:512 + (h + 1) * 128], zrq[:, 4 + h, :], identB[:, :], [zrq.b, identB.b], [pstb])
                        CP("dve", QcT[:, :, :], pst[0:64, 0:512].rearrange("p (h q) -> p h q", h=4), [pstb], [QcT.b])
                        CP("act", QiT[:, :, :], pst[0:64, 512:1024].rearrange("p (h q) -> p h q", h=4), [pstb], [QiT.b])
                        TR(pst[0:64, 0:128], zrk[:, 0, :], identB[:, :], [zrk.b, identB.b], [pstb])
                        TR(pst[0:64, 128:256], zrk[:, 1, :], identB[:, :], [zrk.b, identB.b], [pstb])
                        CP("dve", KcT[:, t * 128:(t + 1) * 128], pst[0:64, 0:128], [pstb], [kcb[t]])
                        CP("act", KiT[:, t * 128:(t + 1) * 128], pst[0:64, 128:256], [pstb], [kib[t]])
                        yield
                        acc = PS[6]

                        def nS():
                            cnt1[0] += 1
                            return PS[4 + cnt1[0] % 2]

                        def nP():
                            cnt1[1] += 1
                            return pTs1[cnt1[1] % 5]
                        if 'a' not in skip:
                            mlist = [m for m in range(5) if t - 4 + m >= 0]
                            pa = {}
                            for m in mlist:
                                sl = (t - 4 + m) % 8
                                sps = nS()
                                for h in range(4):
                                    MM(sps[:, h * 128:(h + 1) * 128], KaT[:, h, sl * 128:(sl + 1) * 128],
                                       QaT[:, h, :], h == 0, False, [KaT.b, QaT.b], [sps.b])
                                MM(sps[:, :], identB[:, :], biasAll[:, l, m * 512:(m + 1) * 512], False, True, [identB.b, biasAll.b], [sps.b])
                                p = nP()
                                ACT(p[:, :], sps[:, :], AF.Exp, [sps.b], [p.b])
                                pa[m] = p
                                yield
                            first = True
                            for h in range(4):
                                for m in mlist:
                                    sl = (t - 4 + m) % 8
                                    MM(acc[:, h * 65:(h + 1) * 65], pa[m][:, h * 128:(h + 1) * 128], Va[:, sl, h, :], first, False,
                                       [pa[m].b, Va.b], [acc.b])
                                    first = False
                            accv = recip_norm(acc, rinv1, yt)
                            TT("dve", yt[:, 0:256].rearrange("p (h d) -> p h d", h=4), accv[:, :, 0:64],
                               rinv1[:, :].unsqueeze(2).to_broadcast([128, 4, 64]), ALU.mult, [acc.b, rinv1.b], [yt.b])
                            yield
                        if 'b' not in skip:
                            def qk_b(j):
                                sps = nS()
                                for h in range(4):
                                    MM(sps[:, h * 128:(h + 1) * 128], KbT[:, h, j * 128:(j + 1) * 128], QbT[:, h, :], h == 0, False,
                                       [KbT.b, QbT.b], [sps.b])
                                if j == t:
                                    MM(sps[:, :], identB[:, :], tribB[:, :], False, True, [identB.b, tribB.b], [sps.b])
                                p = nP()
                                ACT(p[:, :], sps[:, :], AF.Exp, [sps.b], [p.b])
                                return p

                            def pv_b(j, p, first):
                                for h in range(4):
                                    MM(acc[:, h * 65:(h + 1) * 65], p[:, h * 128:(h + 1) * 128], Vb[:, j, h, :], first and h == 0, False,
                                       [p.b, Vb.b], [acc.b])
                            pprev = qk_b(0)
                            for j in range(1, t + 1):
                                pcur = qk_b(j)
                                pv_b(j - 1, pprev, j == 1)
                                pprev = pcur
                                yield
                            pv_b(t, pprev, t == 0)
                            yield
                            accv = recip_norm(acc, rinv1, yt)
                            TT("dve", yt[:, 256:512].rearrange("p (h d) -> p h d", h=4), accv[:, :, 0:64],
                               rinv1[:, :].unsqueeze(2).to_broadcast([128, 4, 64]), ALU.mult, [acc.b, rinv1.b], [yt.b])
                            yield

                    def stage2(t):
                        fo = fos[t % 2]
                        QcT = QcTs[t % 2]
                        QiT = QiTs[t % 2]
                        yt = yts[t % 2]
                        acc = PS[1]

                        def nS():
                            cnt2[0] += 1
                            return PS[2 + cnt2[0] % 2]

                        def nP():
                            cnt2[1] += 1
                            return pTs2[cnt2[1] % 3]
                        if 'c' not in skip:
                            N = 128 * (t + 1)
                            nkb = (N + 511) // 512
                            for kb in range(nkb):
                                cw = min(512, N - kb * 512)
                                kbufs = kib[kb * 4:min(kb * 4 + 4, t + 1)]
                                for h in range(4):
                                    sps = nS()
                                    MM(sps[:, 0:cw], QiT[:, h, :], KiT[:, kb * 512:kb * 512 + cw], True, True, [QiT.b] + kbufs, [sps.b])
                                    rt = rts[h % 2]
                                    ACT(rt[:, 0:cw], sps[:, 0:cw], AF.Relu, [sps.b, fo.b], [rt.b], scale=fo[:, 16 + h:17 + h])
                                    sc = score[:, kb * 512:kb * 512 + cw]
                                    if h == 0:
                                        TS("dve", sc, rt[:, 0:cw], fo[:, 20:21], None, ALU.mult, None, [rt.b, fo.b], [score.b])
                                    else:
                                        STT("dve", sc, rt[:, 0:cw], fo[:, 20 + h:21 + h], sc, ALU.mult, ALU.add, [rt.b, fo.b, score.b], [score.b])
                                    yield
                            if N <= TOPK or 'n' in skip:
                                MS("dve", bis[:, 0:1], -1e29, [bis.b])
                                MS("dve", score[0:64, t * 128 + 64:(t + 1) * 128], -1e30, [score.b])
                            else:
                                pg.op("dve", lambda e, N=N: e.tensor_reduce(out=bis[:, 1:2], in_=score[:, 0:N], axis=AX.X, op=ALU.max), r=[score.b], w=[bis.b])
                                pg.op("dve", lambda e, N=N: e.tensor_reduce(out=bis[:, 2:3], in_=score[:, 0:N], axis=AX.X, op=ALU.min), r=[score.b], w=[bis.b])
                                MS("dve", score[0:64, t * 128 + 64:(t + 1) * 128], -1e30, [score.b])
                                TT("dve", bis[:, 3:4], bis[:, 1:2], bis[:, 2:3], ALU.subtract, [bis.b], [bis.b])
                                TS("dve", bis[:, 8:8 + NIT], pow2, bis[:, 3:4], None, ALU.mult, None, [cst.b, bis.b], [bis.b])
                                TT("dve", bis[:, 4:5], bis[:, 2:3], bis[:, 8:9], ALU.add, [bis.b], [bis.b])
                                yield
                                for it in range(NIT):
                                    TS("dve", MBt[:, 0:N], score[:, 0:N], bis[:, 4:5], None, ALU.is_ge, ALU.add, [score.b, bis.b],
                                       [MBt.b, bis.b], accum_out=bis[:, 5:6])
                                    if it < NIT - 1:
                                        TS("dve", bis[:, 6:7], bis[:, 5:6], TOPK - 0.5, 0.5, ALU.is_ge, ALU.subtract, [bis.b], [bis.b])
                                        STT("dve", bis[:, 4:5], bis[:, 6:7], bis[:, 8 + it:9 + it], bis[:, 4:5], ALU.mult, ALU.add,
                                            [bis.b], [bis.b])
                                    else:
                                        TS("dve", bis[:, 6:7], bis[:, 5:6], TOPK - 0.5, 1.0, ALU.is_ge, ALU.subtract, [bis.b], [bis.b])
                                        STT("dve", bis[:, 0:1], bis[:, 6:7], bis[:, 8 + it:9 + it], bis[:, 4:5], ALU.mult, ALU.add,
                                            [bis.b], [bis.b])
                                    yield
                            TS("dve", MBt[:, 0:N], score[:, 0:N], bis[:, 0:1], NEGM, ALU.is_lt, ALU.mult, [score.b, bis.b], [MBt.b])
                            yield
                            def qk_c(j):
                                sps = nS()
                                MM(sps[:, :], KcT[:, j * 128:(j + 1) * 128], QcT[:, :, :].rearrange("p h q -> p (h q)"), True, False,
                                   [kcb[j], QcT.b], [sps.b])
                                MM(sps[:, :], MBt[:, j * 128:(j + 1) * 128], i4B[:, :], False, True, [MBt.b, i4B.b], [sps.b])
                                p = nP()
                                ACT(p[:, :], sps[:, :], AF.Exp, [sps.b], [p.b])
                                return p

                            def pv_c(j, p, first):
                                for h in range(4):
                                    MM(acc[:, h * 65:(h + 1) * 65], p[:, h * 128:(h + 1) * 128], Vc[:, j, :], first and h == 0, False,
                                       [p.b, vcb[j]], [acc.b])
                            pprev = qk_c(0)
                            for j in range(1, t + 1):
                                pcur = qk_c(j)
                                pv_c(j - 1, pprev, j == 1)
                                pprev = pcur
                                yield
                            pv_c(t, pprev, t == 0)
                            yield
                            accv = recip_norm(acc, rinv2, yt)
                            TT("dve", yt[:, 512:768].rearrange("p (h d) -> p h d", h=4), accv[:, :, 0:64],
                               rinv2[:, :].unsqueeze(2).to_broadcast([128, 4, 64]), ALU.mult, [acc.b, rinv2.b], [yt.b])
                        pst2 = PSB[3]
                        pst2b = PS[3].b
                        for c6 in range(6):
                            TR(pst2[:, c6 * 128:(c6 + 1) * 128], yt[:, c6 * 128:(c6 + 1) * 128], identB[:, :], [yt.b, identB.b], [pst2b])
                        CP("act", yTs[:, :, :], pst2[:, 0:768].rearrange("p (c q) -> p c q", c=6), [pst2b], [yTs.b])
                        DMA("sp", yT_d[s, :, :, t * 128:(t + 1) * 128], yTs[:, :, :], [yTs.b], [], yTs.b)
                        yield

                    def interleave(g1, g2):
                        d1 = g1 is None
                        d2 = g2 is None
                        while not (d1 and d2):
                            if not d1:
                                try:
                                    next(g1)
                                except StopIteration:
                                    d1 = True
                            if not d2:
                                try:
                                    next(g2)
                                except StopIteration:
                                    d2 = True

                    for t in range(NT):
                        interleave(stage1(t), stage2(t - 1) if t >= 1 else None)
                    interleave(None, stage2(NT - 1))
                    pg.barrier()
                    pg.emit()
                if stop_after == "AB":
                    continue
                GC = 512 if S % 512 == 0 else 256
                with ExitStack() as st:
                    wg = sb("wg", [128, KC, 3072], BF16, st)
                    wb = sb("wb", [128, 6, D], BF16, st)
                    wo = sb("wo", [128, KC, D], BF16, st)
                    NQ = GC // 128
                    xt4s = [[sb("xc%d_%d" % (j, i), [128, D], F32, st) for i in range(NQ)] for j in range(2)]
                    xn = sb("xn", [128, D], F32, st)
                    junk = sb("junk", [128, D], BF16, st)
                    hT4s = [sb("hT4_%d" % j, [128, KC, GC], BF16, st) for j in range(2)]
                    yT4s = [sb("yT4_%d" % j, [128, 6, GC], BF16, st) for j in range(2)]
                    gts = [sb("gt%d" % i, [128, GC], F32, st) for i in range(3)]
                    tm = [sb("tm%d" % i, [128, GC], F32, st) for i in range(2)]
                    mT = sb("mT", [128, KC, GC], BF16, st)
                    ots = [sb("ot%d" % i, [128, D], F32, st) for i in range(2)]
                    ss = sb("ss", [128, 8], F32, st)
                    ss2 = sb("ss2", [128, 8], F32, st)
                    junk2 = sb("junk2", [128, D], BF16, st)
                    wsrc = w_in_d[l].rearrange("(k p) n -> p k n", p=128)
                    for br in range(3):
                        DMA("pool", wg[:, :, br * 1024:(br + 1) * 1024], wsrc[:, :, OFF["zg"] + br * 1024:OFF["zg"] + (br + 1) * 1024],
                            [], [wg.b], wg.b)
                    DMA("pool", wb[:, :, :], w_branch_d[l].rearrange("b (k p) n -> p (b k) n", p=128), [], [wb.b], wb.b)
                    DMA("pool", wo[:, :, :], w_out_d[l].rearrange("(k p) n -> p k n", p=128), [], [wo.b], wo.b)
                    G1 = sb("G1", [128, D], F32, st)
                    DMA("sp", G1[:, :], G_d[l, s, 0], [], [G1.b], G1.b)
                    oic = [0]

                    def c_norm(g):
                        t0 = g * GC
                        hT4 = hT4s[g % 2]
                        yT4 = yT4s[g % 2]
                        DMA("sp", yT4[:, :, :], yT_d[s, :, :, t0:t0 + GC], [], [yT4.b], yT4.b)
                        for q in range(NQ):
                            xt = xt4s[g % 2][q]
                            DMA("sp", xt[:, :], xin_d[s, t0 + q * 128:t0 + (q + 1) * 128, :], [], [xt.b], xt.b)
                            norm_hT(xt, xn, junk, ss, (lambda q: lambda k: (hT4[:, k, q * 128:(q + 1) * 128], hT4.b))(q), A1, B1, PS[0], PS[1])
                            yield

                    def c_main(g):
                        t0 = g * GC
                        hT4 = hT4s[g % 2]
                        yT4 = yT4s[g % 2]
                        for fc in range(KC):
                            pps = []
                            for br in range(3):
                                ps = PS[2 + br]
                                for k in range(KC):
                                    MM(ps[:, 0:GC], wg[:, k, br * 1024 + fc * 128:br * 1024 + (fc + 1) * 128], hT4[:, k, :], k == 0, k == KC - 1,
                                       [wg.b, hT4.b], [ps.b])
                                ACT(gts[br][:, :], ps[:, 0:GC], AF.Sigmoid, [ps.b, small.b], [gts[br].b], bias=b_gateT[:, l, br, fc:fc + 1])
                                pp = PS[5 + br]
                                for kk in range(2):
                                    MM(pp[:, 0:GC], wb[:, br * 2 + kk, fc * 128:(fc + 1) * 128], yT4[:, br * 2 + kk, :], kk == 0, kk == 1,
                                       [wb.b, yT4.b], [pp.b])
                                pps.append(pp)
                            TT("dve", tm[0][:, :], gts[0][:, :], pps[0][:, 0:GC], ALU.mult, [gts[0].b, pps[0].b], [tm[0].b])
                            TT("dve", tm[1][:, :], gts[1][:, :], pps[1][:, 0:GC], ALU.mult, [gts[1].b, pps[1].b], [tm[1].b])
                            TT("pool", tm[0][:, :], tm[0][:, :], tm[1][:, :], ALU.add, [tm[0].b, tm[1].b], [tm[0].b])
                            TT("dve", tm[1][:, :], gts[2][:, :], pps[2][:, 0:GC], ALU.mult, [gts[2].b, pps[2].b], [tm[1].b])
                            TT("pool", mT[:, fc, :], tm[0][:, :], tm[1][:, :], ALU.add, [tm[0].b, tm[1].b], [mT.b])
                            yield
                        for q in range(NQ):
                            for half in range(2):
                                ps = PS[2 + half]
                                for k in range(KC):
                                    MM(ps[:, :], mT[:, k, q * 128:(q + 1) * 128], wo[:, k, half * 512:(half + 1) * 512], k == 0, k == KC - 1,
                                       [mT.b, wo.b], [ps.b])
                            ot = ots[oic[0] % 2]
                            oic[0] += 1
                            postnorm_residual(PS[2], PS[3], xt4s[g % 2][q], G1, ss2, ot, junk2)
                            DMA("sp", x1_d[s, t0 + q * 128:t0 + (q + 1) * 128, :], ot[:, :], [ot.b], [], ot.b)
                            yield

                    NGC = S // GC
                    interleave(c_norm(0), None)
                    for g in range(NGC):
                        interleave(c_main(g), c_norm(g + 1) if g + 1 < NGC else None)
                    pg.barrier()
                    pg.emit()
                if stop_after == "C":
                    continue
                with ExitStack() as st:
                    wu = sb("wu", [128, KC, 2 * DFF], BF16, st)
                    wd = sb("wd", [128, FC, D], BF16, st)
                    xts = [sb("xd%d" % i, [128, D], F32, st) for i in range(1)]
                    xrs = [sb("xr%d" % i, [128, D], F32, st) for i in range(1)]
                    xn = sb("xn", [128, D], F32, st)
                    junk = sb("junk", [128, D], BF16, st)
                    junk2 = sb("junk2", [128, D], BF16, st)
                    hT2s = [sb("hT2_%d" % j, [128, KC, GD], BF16, st) for j in range(2)]
                    actT = sb("actT", [128, FC, GD], BF16, st)
                    araw = [sb("araw%d" % i, [128, GD + 2], F32, st) for i in range(2)]
                    cv = [sb("cv%d" % i, [128, GD], F32, st) for i in range(2)]
                    ge = [sb("ge%d" % i, [128, GD], F32, st) for i in range(2)]
                    halo = sb("halo", [128, FC, 2], F32, st)
                    gsb = [sb("gs%d" % i, [128, GD], F32, st) for i in range(2)]
                    ots = [sb("ot%d" % i, [128, D], F32, st) for i in range(2)]
                    ss = sb("ss", [128, 8], F32, st)
                    ss2 = sb("ss2", [128, 8], F32, st)
                    usrc = w_up_d[l].rearrange("(k p) n -> p k n", p=128)
                    for c0 in range(0, 2 * DFF, 1408):
                        DMA("pool", wu[:, :, c0:c0 + 1408], usrc[:, :, c0:c0 + 1408], [], [wu.b], wu.b)
                    dsrc = w_down_d[l].rearrange("(k p) n -> p k n", p=128)
                    for k0 in range(0, FC, 11):
                        DMA("pool", wd[:, k0:k0 + 11, :], dsrc[:, k0:k0 + 11, :], [], [wd.b], wd.b)
                    MS("dve", halo[:, :, :], 0.0, [halo.b])
                    G2 = sb("G2", [128, D], F32, st)
                    DMA("sp", G2[:, :], G_d[l, s, 1], [], [G2.b], G2.b)
                    oid = [0]

                    def d_norm(g):
                        t0 = g * GD
                        hT2 = hT2s[g % 2]
                        for q in range(GD // 128):
                            xt = xts[0]
                            DMA("sp", xt[:, :], x1_d[s, t0 + q * 128:t0 + (q + 1) * 128, :], [], [xt.b], xt.b)
                            norm_hT(xt, xn, junk, ss, (lambda q: lambda k: (hT2[:, k, q * 128:(q + 1) * 128], hT2.b))(q), A2, B2, PS[0], PS[1])
                            yield

                    def d_main(g):
                        t0 = g * GD
                        hT2 = hT2s[g % 2]
                        for fc in range(FC):
                            pa_ = PS[2 + (fc % 2) * 2]
                            pgt = PS[3 + (fc % 2) * 2]
                            for k in range(KC):
                                MM(pa_[:, 0:GD], wu[:, k, fc * 128:(fc + 1) * 128], hT2[:, k, :], k == 0, k == KC - 1, [wu.b, hT2.b], [pa_.b])
                            for k in range(KC):
                                MM(pgt[:, 0:GD], wu[:, k, DFF + fc * 128:DFF + (fc + 1) * 128], hT2[:, k, :], k == 0, k == KC - 1,
                                   [wu.b, hT2.b], [pgt.b])
                            ar = araw[fc % 2]
                            c_ = cv[fc % 2]
                            g_ = ge[fc % 2]
                            gs_ = gsb[fc % 2]
                            CP("dve", gs_[:, :], pgt[:, 0:GD], [pgt.b], [gs_.b])
                            CP("pool", ar[:, 0:2], halo[:, fc, :], [halo.b], [ar.b])
                            CP("act", ar[:, 2:GD + 2], pa_[:, 0:GD], [pa_.b], [ar.b])
                            CP("pool", halo[:, fc, :], ar[:, GD:GD + 2], [ar.b], [halo.b])
                            ACT(c_[:, :], ar[:, 2:GD + 2], AF.Identity, [ar.b, small.b], [c_.b], bias=conv_bT[:, l, fc:fc + 1],
                                scale=conv_wT[:, l, 2, fc:fc + 1])
                            STT("dve", c_[:, :], ar[:, 1:GD + 1], conv_wT[:, l, 1, fc:fc + 1], c_[:, :], ALU.mult, ALU.add, [ar.b, small.b, c_.b], [c_.b])
                            STT("dve", c_[:, :], ar[:, 0:GD], conv_wT[:, l, 0, fc:fc + 1], c_[:, :], ALU.mult, ALU.add, [ar.b, small.b, c_.b], [c_.b])
                            ACT(g_[:, :], c_[:, :], AF.Gelu_apprx_tanh, [c_.b], [g_.b])
                            TT("dve", actT[:, fc, :], g_[:, :], gs_[:, :], ALU.mult, [g_.b, gs_.b], [actT.b])
                            yield
                        for q in range(GD // 128):
                            xr = xrs[0]
                            DMA("sp", xr[:, :], x1_d[s, t0 + q * 128:t0 + (q + 1) * 128, :], [], [xr.b], xr.b)
                            for half in range(2):
                                ps = PS[6 + half]
                                for k in range(FC):
                                    MM(ps[:, :], actT[:, k, q * 128:(q + 1) * 128], wd[:, k, half * 512:(half + 1) * 512], k == 0, k == FC - 1,
                                       [actT.b, wd.b], [ps.b])
                            ot = ots[oid[0] % 2]
                            oid[0] += 1
                            postnorm_residual(PS[6], PS[7], xr, G2, ss2, ot, junk2)
                            DMA("sp", xout_d[s, t0 + q * 128:t0 + (q + 1) * 128, :], ot[:, :], [ot.b], [], ot.b)
                            yield

                    NGD = S // GD
                    interleave(d_norm(0), None)
                    for g in range(NGD):
                        interleave(d_main(g), d_norm(g + 1) if g + 1 < NGD else None)
                    pg.barrier()
                    pg.emit()
    return nc


def _consts(S):
    NT = S // 128
    cst = np.zeros((128, NCST), np.float32)
    k = np.arange(128)[:, None]
    m = np.arange(128)[None, :]
    cst[:, 0:128] = (k == m)
    cst[:, 128:256] = (k <= m)
    cst[:, 256:384] = (k == 127)
    cst[:, 384:896] = np.tile((k == m).astype(np.float32), (1, 4))
    cst[:, 896:1408] = np.tile(np.where(k > m, NEGM, 0.0).astype(np.float32), (1, 4))
    cst[:, 1408:1408 + NIT] = (2.0 ** -(np.arange(NIT) + 1.0))[None, :]
    pos = np.arange(S, dtype=np.float32)
    inv_freq = (np.float32(10000.0) ** (-np.arange(0, 64, 2, dtype=np.float32) / np.float32(64))).astype(np.float32)
    ang = (pos[:, None] * inv_freq[None, :]).astype(np.float32)
    cos = np.cos(ang).astype(np.float32)
    sin = np.sin(ang).astype(np.float32)
    cs = np.concatenate([cos, sin, cos * np.float32(0.125), sin * np.float32(0.125)], axis=1).reshape(NT, 128, 128)
    ki = np.arange(128)[:, None, None]
    mm = np.arange(5)[None, :, None]
    qi = np.arange(128)[None, None, :]
    rel = 512 - 128 * mm + qi - ki
    qm = qi % 64
    valid = (rel >= qm - 63) & (rel <= qm + 512)
    relidx = np.clip(rel, -128, 128) + 128
    maskA = np.where(valid, 0.0, NEGM).astype(np.float32)
    maskA = np.broadcast_to(maskA[:, :, None, :], (128, 5, 4, 128)).reshape(128, 5 * 512)
    return cst, cs.astype(np.float32), relidx, np.ascontiguousarray(maskA)


def _prep(inputs, S, NSEQ, ncores):
    f = lambda a: np.ascontiguousarray(np.asarray(a, dtype=np.float32))
    cst, cs, relidx, maskA = _consts(S)
    x = f(inputs["x"])
    c = f(inputs["c"])
    b_ada = f(inputs["b_ada"])
    norm_g = f(inputs["norm_g"])
    rel_table = f(inputs["rel_table"])
    shared = {
        "w_ada": f(inputs["w_ada"]),
        "b_adaT": np.ascontiguousarray(b_ada.reshape(2, 48, 128).transpose(2, 0, 1)),
        "b_adaB": np.ascontiguousarray(np.broadcast_to(
            b_ada.reshape(2, 6, D)[:, [2, 5], :][None], (128, 2, 2, D))),
        "normgT": np.ascontiguousarray(norm_g.reshape(2, 4, KC, 128).transpose(3, 0, 1, 2)),
        "normgB": np.ascontiguousarray(np.broadcast_to(norm_g[:, [1, 3], :][None], (128, 2, 2, D))),
        "w_in": f(inputs["w_in"]),
        "b_gateT": np.ascontiguousarray(f(inputs["b_gate"]).reshape(2, 3, KC, 128).transpose(3, 0, 1, 2)),
        "biasT": np.ascontiguousarray(rel_table[:, :, relidx].transpose(0, 2, 3, 1, 4).reshape(2, 128, 5 * 512)),
        "maskA": maskA,
        "b_forgetB": np.ascontiguousarray(np.broadcast_to(f(inputs["b_forget"])[None], (128, 2, 4))),
        "w_branch": f(inputs["w_branch"]),
        "w_out": f(inputs["w_out"]),
        "w_up": f(inputs["w_up"]),
        "conv_wT": np.ascontiguousarray(f(inputs["conv_w"]).reshape(2, 3, FC, 128).transpose(3, 0, 1, 2)),
        "conv_bT": np.ascontiguousarray(f(inputs["conv_b"]).reshape(2, FC, 128).transpose(2, 0, 1)),
        "w_down": f(inputs["w_down"]),
        "cs": cs,
        "cst": cst,
    }
    maps = []
    for i in range(ncores):
        d = dict(shared)
        d["x"] = np.ascontiguousarray(x[i * NSEQ:(i + 1) * NSEQ])
        cc = c[i * NSEQ:(i + 1) * NSEQ]
        d["cT"] = np.ascontiguousarray(cc.reshape(NSEQ, KC, 128).transpose(2, 1, 0))
        maps.append(d)
    return maps


def run(inputs, cfg, ncores=8):
    nc = build(cfg)
    maps = _prep(inputs, cfg["S"], cfg["NSEQ"], ncores)
    res = run_bass_kernel_spmd(nc, maps, core_ids=list(range(ncores)))
    return res


def kernel(**inputs):
    x = np.asarray(inputs["x"])
    B, S, _ = x.shape
    ncores = 8
    NSEQ = B // ncores
    cfg = dict(S=S, NSEQ=NSEQ, NL=2)
    res = run(inputs, cfg, ncores)
    out = np.concatenate([np.asarray(r["out"]) for r in res.results], axis=0)
    return out.astype(np.float32)
```

```python
import numpy as np
from contextlib import ExitStack
import concourse.bass as bass
import concourse.mybir as mybir
from concourse.bass_utils import run_bass_kernel_spmd

F32 = mybir.dt.float32
BF16 = mybir.dt.bfloat16
AF = mybir.ActivationFunctionType
ALU = mybir.AluOpType
AX = mybir.AxisListType

D = 1024
KC = 8
NIN = 5320
DFF = 2816
FC = 22
NEGM = -30000.0
NIT = 12
EPS = 1e-6
NCST = 128 * 3 + 512 + 512 + NIT

OFF = dict(qa=0, ka=256, va=512, qb=768, kb=1024, vb=1280, fb=1536, qc=1540, kc=1796, vc=1860,
           qi=1924, ki=2180, wi=2244, zg=2248)
DST = dict(qa=0, ka=256, qb=512, kb=768, va=1024, vb=1280, qc=1536, qi=1792,
           kc=2048, ki=2112, vc=2176, fb=2240, wi=2244)
WID = dict(qa=256, ka=256, qb=256, kb=256, va=256, vb=256, qc=256, qi=256, kc=64, ki=64, vc=64, fb=4, wi=4)
NQKV = 2248


class Buf:
    __slots__ = ("name", "w", "r", "rd", "sem", "cnt", "excl")

    def __init__(self, name=""):
        self.name = name
        self.excl = False
        self.w = None
        self.r = {}
        self.rd = []
        self.sem = None
        self.cnt = 0


class Prog:
    ENG = ("pe", "act", "dve", "pool", "sp")

    def __init__(self, nc, stack):
        self.nc = nc
        self.stack = stack
        self.ops = []
        self.emitted = 0
        self.esem = {e: stack.enter_context(nc.semaphore("es_" + e)) for e in ("pe", "act", "dve", "pool")}
        self.ecnt = {e: 0 for e in self.esem}
        self.seen = {e: {} for e in self.ENG}
        self.tok = Buf("tok")
        self.nsem = 0
        self.last_bar = None
        self.free_sems = []
        self.sem_bufs = []

    def dsem(self, buf, fresh=False):
        if buf.sem is None:
            if fresh:
                self.nsem += 1
                buf.sem = self.stack.enter_context(self.nc.semaphore("dq%d" % self.nsem))
                buf.cnt = 0
                return buf.sem
            if self.free_sems:
                buf.sem, buf.cnt = self.free_sems.pop()
            else:
                self.nsem += 1
                buf.sem = self.stack.enter_context(self.nc.semaphore("ds%d" % self.nsem))
                buf.cnt = 0
            self.sem_bufs.append(buf)
        return buf.sem

    def release_sems(self):
        for b in self.sem_bufs:
            self.free_sems.append((b.sem, b.cnt))
            b.sem = None
            for e in self.ENG:
                self.seen[e].pop(id(b), None)
        self.sem_bufs = []

    def op(self, eng, fn, r=(), w=(), dma=None, tok=True):
        deps = set()
        w = list(w) + [b for b in r if b.excl and b not in w]
        r = [b for b in r if not b.excl]
        if tok:
            r.append(self.tok)
        for b in r:
            if b.w is not None:
                deps.add(b.w)
        for b in w:
            if b.w is not None:
                wo = self.ops[b.w]
                if not (dma is not None and wo["dma"] is dma and wo["eng"] == eng):
                    deps.add(b.w)
            deps.update(b.r.values())
            deps.update(b.rd)
        if tok and self.last_bar is not None:
            deps = {d for d in deps if d >= self.last_bar}
        i = len(self.ops)
        o = dict(eng=eng, fn=fn, deps=deps, dma=dma, needed=False, waits=None, cnt=None, dval=None)
        if dma is not None:
            self.dsem(dma, fresh=(eng == "pool"))
            dma.cnt += 16
            o["dval"] = dma.cnt
        self.ops.append(o)
        for b in r:
            if dma is not None:
                b.rd.append(i)
            else:
                b.r[eng] = i
        for b in w:
            b.w = i
            b.r = {}
            b.rd = []
        return i

    def barrier(self):
        self.last_bar = self.op("dve", lambda e: e.memset(self.bar_tile, 0.0), w=[self.tok], tok=False)
        self.ops[self.last_bar]["needed"] = True

    def emit(self):
        nc = self.nc
        ops = self.ops[self.emitted:]
        for o in ops:
            eng = o["eng"]
            seen = self.seen[eng]
            per = {}
            dmaw = {}
            for d in o["deps"]:
                do = self.ops[d]
                if do["dma"] is not None:
                    key = id(do["dma"])
                    if do["dval"] > seen.get(key, 0):
                        if key not in dmaw or dmaw[key][1] < do["dval"]:
                            dmaw[key] = (do["dma"], do["dval"])
                else:
                    f = do["eng"]
                    if f == "pe" and eng == "pe":
                        continue
                    if d > seen.get(f, -1):
                        per[f] = max(per.get(f, -1), d)
            w = []
            for f, d in per.items():
                seen[f] = d
                self.ops[d]["needed"] = True
                w.append(("e", f, d))
            for key, (buf, val) in dmaw.items():
                seen[key] = val
                w.append(("d", buf, val))
            o["waits"] = w
        for o in ops:
            if o["dma"] is None and o["needed"]:
                self.ecnt[o["eng"]] += 1
                o["cnt"] = self.ecnt[o["eng"]]
        per_eng = {e: [] for e in self.ENG}
        for o in ops:
            per_eng[o["eng"]].append(o)

        def run(eng_name):
            def body(e):
                for o in per_eng[eng_name]:
                    for wt in o["waits"]:
                        if wt[0] == "e":
                            e.wait_ge(self.esem[wt[1]], self.ops[wt[2]]["cnt"])
                        else:
                            e.wait_ge(wt[1].sem, wt[2])
                    if o["fn"] is None:
                        continue
                    ins = o["fn"](e)
                    if o["dma"] is not None:
                        ins.then_inc(o["dma"].sem, 16)
                    elif o["needed"]:
                        ins.then_inc(self.esem[eng_name], 1)
            return body

        with nc.Block() as block:
            block.tensor(run("pe"))
            block.scalar(run("act"))
            block.vector(run("dve"))
            block.gpsimd(run("pool"))
            block.sync(run("sp"))
        self.emitted = len(self.ops)
        if self.last_bar == len(self.ops) - 1:
            self.release_sems()


class T:
    def __init__(self, ap, name=""):
        self.ap = ap
        self.b = Buf(name)

    def __getitem__(self, k):
        return self.ap[k]


def build(cfg):
    S = cfg["S"]
    NSEQ = cfg["NSEQ"]
    NL = cfg["NL"]
    NT = S // 128
    GD = 256
    TOPK = min(256, S // 4)
    stop_after = cfg.get("stop_after", "D")
    dbg = cfg.get("dbg", False)
    skip = cfg.get("skip", "")

    nc = bass.Bass("TRN2", target_bir_lowering=False)

    def din(name, shape, dt=F32):
        return nc.dram_tensor(name, list(shape), dt, kind="ExternalInput").ap()

    x_d = din("x", [NSEQ, S, D])
    cT_d = din("cT", [128, KC, NSEQ])
    w_ada_d = din("w_ada", [2, D, 6 * D])
    b_adaT_d = din("b_adaT", [128, 2, 48])
    b_adaB_d = din("b_adaB", [128, 2, 2, D])
    normgT_d = din("normgT", [128, 2, 4, KC])
    normgB_d = din("normgB", [128, 2, 2, D])
    w_in_d = din("w_in", [2, D, NIN])
    b_gateT_d = din("b_gateT", [128, 2, 3, KC])
    biasT_d = din("biasT", [2, 128, 5 * 512])
    maskA_d = din("maskA", [128, 5 * 512])
    b_forgetB_d = din("b_forgetB", [128, 2, 4])
    w_branch_d = din("w_branch", [2, 3, 256, D])
    w_out_d = din("w_out", [2, D, D])
    w_up_d = din("w_up", [2, D, 2 * DFF])
    conv_wT_d = din("conv_wT", [128, 2, 3, FC])
    conv_bT_d = din("conv_bT", [128, 2, FC])
    w_down_d = din("w_down", [2, DFF, D])
    cs_d = din("cs", [NT, 128, 128])
    cst_d = din("cst", [128, NCST])
    out_d = nc.dram_tensor("out", [NSEQ, S, D], F32, kind="ExternalOutput").ap()
    xs_d = [nc.dram_tensor("xs%d" % i, [NSEQ, S, D], F32, kind="Internal").ap() for i in range(2)]
    yT_d = nc.dram_tensor("yT", [NSEQ, 128, 6, S], BF16, kind="ExternalOutput" if dbg else "Internal").ap()

    stack = ExitStack()
    with stack:
        pg = Prog(nc, stack)

        uid = [0]

        def sb(name, shape, dt, st=stack):
            uid[0] += 1
            return T(st.enter_context(nc.sbuf_tensor("%s_%d" % (name, uid[0]), list(shape), dt)), name)

        def MM(out, lhsT, rhs, start, stop, r, w):
            pg.op("pe", lambda e: e.matmul(out=out, lhsT=lhsT, rhs=rhs, start=start, stop=stop,
                                           skip_group_check=True), r=r, w=w)

        def TR(out, in_, ident, r, w):
            pg.op("pe", lambda e: e.transpose(out=out, in_=in_, identity=ident), r=r, w=w)

        def ACT(out, in_, func, r, w, bias=None, scale=None, accum_out=None):
            kw = {}
            if bias is not None:
                kw["bias"] = bias
            if scale is not None:
                kw["scale"] = scale
            if accum_out is not None:
                kw["accum_out"] = accum_out
            pg.op("act", lambda e: e.activation(out=out, in_=in_, func=func, **kw), r=r, w=w)

        def TS(eng, out, in0, s1, s2, op0, op1, r, w, accum_out=None):
            kw = {}
            if op1 is not None:
                kw["op1"] = op1
            if accum_out is not None:
                kw["accum_out"] = accum_out
            pg.op(eng, lambda e: e.tensor_scalar(out=out, in0=in0, scalar1=s1, scalar2=s2, op0=op0, **kw), r=r, w=w)

        def TT(eng, out, in0, in1, op, r, w):
            pg.op(eng, lambda e: e.tensor_tensor(out=out, in0=in0, in1=in1, op=op), r=r, w=w)

        def STT(eng, out, in0, scalar, in1, op0, op1, r, w):
            pg.op(eng, lambda e: e.scalar_tensor_tensor(out=out, in0=in0, scalar=scalar, in1=in1, op0=op0, op1=op1), r=r, w=w)

        def CP(eng, out, in_, r, w):
            if eng == "act":
                pg.op("act", lambda e: e.copy(out=out, in_=in_), r=r, w=w)
            else:
                pg.op(eng, lambda e: e.tensor_copy(out=out, in_=in_), r=r, w=w)

        def MS(eng, ap, val, w):
            pg.op(eng, lambda e: e.memset(ap, val), w=w)

        def DMA(eng, out, in_, r, w, sem):
            pg.op(eng, lambda e: e.dma_start(out=out, in_=in_), r=r, w=w, dma=sem)

        bar = sb("bar", [128, 8], F32)
        pg.bar_tile = bar[:, 0:1]
        PS = [T(stack.enter_context(nc.psum_tensor("ps%d" % i, [128, 512], F32)), "ps%d" % i) for i in range(8)]
        PSB = [p[:, :].bitcast(BF16) for p in PS]
        for p in PS:
            p.b.excl = True

        cst = sb("cst", [128, NCST], F32)
        identF = cst[:, 0:128]
        triF = cst[:, 128:256]
        sel127 = cst[:, 256:384]
        i4F = cst[:, 384:896]
        tribF = cst[:, 896:1408]
        pow2 = cst[:, 1408:1408 + NIT]
        identB = sb("identB", [128, 128], BF16)
        i4B = sb("i4B", [128, 512], BF16)
        tribB = sb("tribB", [128, 512], BF16)
        DMA("sp", cst[:, :], cst_d[:, :], [], [cst.b], cst.b)
        CP("dve", identB[:, :], identF, [cst.b], [identB.b])
        CP("dve", i4B[:, :], i4F, [cst.b], [i4B.b])
        CP("dve", tribB[:, :], tribF, [cst.b], [tribB.b])

        NSM = 96 + 64 + 48 + 8 + 6 * FC + 2 * FC + KC * NSEQ
        small = sb("small", [128, NSM], F32)
        o = 0
        b_adaT = small[:, o:o + 96].rearrange("p (l c) -> p l c", l=2); o += 96
        normgT = small[:, o:o + 64].rearrange("p (l j k) -> p l j k", l=2, j=4); o += 64
        b_gateT = small[:, o:o + 48].rearrange("p (l j k) -> p l j k", l=2, j=3); o += 48
        b_forgetB = small[:, o:o + 8].rearrange("p (l h) -> p l h", l=2); o += 8
        conv_wT = small[:, o:o + 6 * FC].rearrange("p (l j f) -> p l j f", l=2, j=3); o += 6 * FC
        conv_bT = small[:, o:o + 2 * FC].rearrange("p (l f) -> p l f", l=2); o += 2 * FC
        cT = small[:, o:o + KC * NSEQ].rearrange("p (k b) -> p k b", k=KC); o += KC * NSEQ
        for dst, src in ((b_adaT, b_adaT_d), (normgT, normgT_d), (b_gateT, b_gateT_d), (b_forgetB, b_forgetB_d),
                         (conv_wT, conv_wT_d), (conv_bT, conv_bT_d), (cT, cT_d)):
            DMA("sp", dst, src, [], [small.b], small.b)
        cact = sb("cact", [128, KC, NSEQ], F32)
        csig = sb("csig", [128, KC, NSEQ], F32)
        ACT(csig[:, :, :], cT, AF.Sigmoid, [small.b], [csig.b])
        TT("dve", cact[:, :, :], cT, csig[:, :, :], ALU.mult, [small.b, csig.b], [cact.b])
        modT = sb("modT", [128, NL, NSEQ, 4, KC], F32)
        G_d = nc.dram_tensor("Gscr", [NL, NSEQ, 2, 128, D], F32, kind="Internal").ap()
        biasAll = sb("biasAll", [128, NL, 5 * 512], BF16)

        with ExitStack() as st0:
            wad = [sb("wad%d" % i, [128, KC, 1024], F32, st0) for i in range(2)]
            cactB = sb("cactB", [128, NSEQ, KC, 128], F32, st0)
            gtl = [sb("gtl%d" % i, [128, D], F32, st0) for i in range(2)]
            biasF = sb("biasF", [128, 5 * 512], F32, st0)
            maskF = sb("maskF", [128, 5 * 512], F32, st0)
            DMA("sp", maskF[:, :], maskA_d[:, :], [], [maskF.b], maskF.b)
            for l in range(NL):
                DMA("sp", biasF[:, :], biasT_d[l], [], [biasF.b], biasF.b)
                TT("dve", biasAll[:, l, :], biasF[:, :], maskF[:, :], ALU.add, [biasF.b, maskF.b], [biasAll.b])
            for s in range(NSEQ):
                CP("dve", cactB[:, s, :, :], cact[:, :, s:s + 1].to_broadcast([128, KC, 128]), [cact.b], [cactB.b])
            gi = 0
            badaB = sb("badaB", [128, D], F32, st0)
            gB = sb("gB", [128, D], F32, st0)
            mtmp = sb("mtmp", [128, NSEQ], F32, st0)
            for l in range(NL):
                for sec in range(6):
                    wt = wad[(l * 6 + sec) % 2]
                    src = w_ada_d[l].rearrange("(k p) n -> p k n", p=128)[:, :, sec * 1024:(sec + 1) * 1024]
                    for k0 in range(0, KC, 2):
                        DMA("sp", wt[:, k0:k0 + 2, :], src[:, k0:k0 + 2, :], [], [wt.b], wt.b)
                    if sec in (2, 5):
                        j = 0 if sec == 2 else 1
                        DMA("sp", badaB[:, :], b_adaB_d[:, l, j, :], [], [badaB.b], badaB.b)
                        DMA("sp", gB[:, :], normgB_d[:, l, j, :], [], [gB.b], gB.b)
                        for s in range(NSEQ):
                            g = gtl[gi % 2]
                            gi += 1
                            for half in range(2):
                                ps = PS[half]
                                for k in range(KC):
                                    MM(ps[:, :], cactB[:, s, k, :], wt[:, k, half * 512:(half + 1) * 512],
                                       k == 0, k == KC - 1, [cactB.b, wt.b], [ps.b])
                                hs = slice(half * 512, (half + 1) * 512)
                                TT("dve", g[:, hs], ps[:, :], badaB[:, hs], ALU.add, [ps.b, badaB.b], [g.b])
                                TT("dve", g[:, hs], g[:, hs], gB[:, hs], ALU.mult, [g.b, gB.b], [g.b])
                            DMA("sp", G_d[l, s, j], g[:, :], [g.b], [], g.b)
                    else:
                        jj = {0: 1, 1: 0, 3: 3, 4: 2}[sec]
                        for kc in range(KC):
                            ps = PS[2 + kc % 2]
                            for k in range(KC):
                                MM(ps[:, 0:NSEQ], wt[:, k, kc * 128:(kc + 1) * 128], cact[:, k, :],
                                   k == 0, k == KC - 1, [cact.b, wt.b], [ps.b])
                            cc = sec * 8 + kc
                            if sec in (0, 3):
                                TS("dve", modT[:, l, :, jj, kc], ps[:, 0:NSEQ], b_adaT[:, l, cc:cc + 1], None, ALU.add, None,
                                   [ps.b, small.b], [modT.b])
                            else:
                                ng = 0 if sec == 1 else 2
                                TS("dve", mtmp[:, :], ps[:, 0:NSEQ], b_adaT[:, l, cc:cc + 1], 1.0, ALU.add, ALU.add,
                                   [ps.b, small.b], [mtmp.b])
                                TS("dve", modT[:, l, :, jj, kc], mtmp[:, :], normgT[:, l, ng, kc:kc + 1], None, ALU.mult, None,
                                   [mtmp.b, small.b], [modT.b])
            pg.barrier()
            pg.emit()

        def norm_hT(xt, xn, junk, ss, hT_ap_fn, A, Bv, psA, psB, extra_r=()):
            ACT(junk[:, 0:D], xt[:, :], AF.Square, [xt.b], [junk.b, ss.b], accum_out=ss[:, 0:1])
            ACT(ss[:, 1:2], ss[:, 0:1], AF.Sqrt, [ss.b], [ss.b], bias=EPS, scale=1.0 / D)
            pg.op("dve", lambda e: e.reciprocal(out=ss[:, 2:3], in_=ss[:, 1:2]), r=[ss.b], w=[ss.b])
            ACT(xn[:, :], xt[:, :], AF.Copy, [xt.b, ss.b], [xn.b], scale=ss[:, 2:3])
            for half, ps in ((0, psA), (1, psB)):
                for kk in range(4):
                    k = half * 4 + kk
                    TR(ps[:, kk * 128:(kk + 1) * 128], xn[:, k * 128:(k + 1) * 128], identF, [xn.b, cst.b], [ps.b])
                for kk in range(4):
                    k = half * 4 + kk
                    outap, wb = hT_ap_fn(k)
                    if kk % 2 == 0:
                        ACT(outap, ps[:, kk * 128:(kk + 1) * 128], AF.Identity, [ps.b, modT.b], [wb],
                            bias=Bv[:, k:k + 1], scale=A[:, k:k + 1])
                    else:
                        TS("dve", outap, ps[:, kk * 128:(kk + 1) * 128], A[:, k:k + 1], Bv[:, k:k + 1], ALU.mult, ALU.add,
                           [ps.b, modT.b], [wb])

        def postnorm_residual(psA, psB, xres, G, ss, ot, junk):
            ACT(junk[:, 0:512], psA[:, :], AF.Square, [psA.b], [junk.b, ss.b], accum_out=ss[:, 4:5])
            ACT(junk[:, 512:1024], psB[:, :], AF.Square, [psB.b], [junk.b, ss.b], accum_out=ss[:, 5:6])
            TT("dve", ss[:, 6:7], ss[:, 4:5], ss[:, 5:6], ALU.add, [ss.b], [ss.b])
            ACT(ss[:, 3:4], ss[:, 6:7], AF.Sqrt, [ss.b], [ss.b], bias=EPS, scale=1.0 / D)
            pg.op("dve", lambda e: e.reciprocal(out=ss[:, 7:8], in_=ss[:, 3:4]), r=[ss.b], w=[ss.b])
            for half, ps in ((0, psA), (1, psB)):
                hs = slice(half * 512, (half + 1) * 512)
                STT("dve", ot[:, hs], ps[:, :], ss[:, 7:8], G[:, hs], ALU.mult, ALU.mult, [ps.b, ss.b, G.b], [ot.b])
                TT("pool", ot[:, hs], ot[:, hs], xres[:, hs], ALU.add, [ot.b, xres.b], [ot.b])

        def interleave(g1, g2):
            d1 = g1 is None
            d2 = g2 is None
            while not (d1 and d2):
                if not d1:
                    try:
                        next(g1)
                    except StopIteration:
                        d1 = True
                if not d2:
                    try:
                        next(g2)
                    except StopIteration:
                        d2 = True

        for l in range(NL if stop_after != "0" else 0):
            xin_d = x_d if l == 0 else xs_d[1]
            x1_d = xs_d[0]
            xout_d = out_d if l == NL - 1 else xs_d[1]
            for s in range(NSEQ):
                A1 = modT[:, l, s, 0, :]
                B1 = modT[:, l, s, 1, :]
                A2 = modT[:, l, s, 2, :]
                B2 = modT[:, l, s, 3, :]
                with ExitStack() as st:
                    wq = sb("wq", [128, KC, NQKV], BF16, st)
                    KaT = sb("KaT", [64, 4, 1024], BF16, st)
                    Va = sb("Va", [128, 8, 4, 65], BF16, st)
                    KbT = sb("KbT", [70, 4, S], BF16, st)
                    Vb = sb("Vb", [128, NT, 4, 65], BF16, st)
                    KcT = sb("KcT", [64, S], BF16, st)
                    KiT = sb("KiT", [64, S], BF16, st)
                    Vc = sb("Vc", [128, NT, 65], BF16, st)
                    kcb = [Buf("kc%d" % i) for i in range(NT)]
                    kib = [Buf("ki%d" % i) for i in range(NT)]
                    vcb = [Buf("vc%d" % i) for i in range(NT)]
                    xts = [sb("xt%d" % i, [128, D], F32, st) for i in range(2)]
                    xn = sb("xn", [128, D], F32, st)
                    hT = sb("hT", [128, KC, 128], BF16, st)
                    css = [sb("cs%d" % i, [128, 128], F32, st) for i in range(2)]
                    zqa = sb("zqa", [128, 256], BF16, st)
                    zka = sb("zka", [128, 256], BF16, st)
                    zqb = sb("zqb", [128, 4, 70], BF16, st)
                    zkb = sb("zkb", [128, 4, 70], BF16, st)
                    zrq = sb("zrq", [128, 8, 64], BF16, st)
                    zrk = sb("zrk", [128, 2, 64], BF16, st)
                    rt1 = sb("rt1", [128, 8, 32], F32, st)
                    rt2 = sb("rt2", [128, 8, 32], F32, st)
                    QaT = sb("QaT", [64, 4, 128], BF16, st)
                    QbT = sb("QbT", [70, 4, 128], BF16, st)
                    QcTs = [sb("QcT%d" % i, [64, 4, 128], BF16, st) for i in range(2)]
                    QiTs = [sb("QiT%d" % i, [64, 4, 128], BF16, st) for i in range(2)]
                    fos = [sb("fo%d" % i, [128, 64], F32, st) for i in range(2)]
                    fob = sb("fob", [128, 16], BF16, st)
                    cum = [sb("cum%d" % i, [128, 4], F32, st) for i in range(2)]
                    ss = sb("ss", [128, 8], F32, st)
                    bis = sb("bis", [128, 8 + NIT], F32, st)
                    score = sb("score", [128, S], F32, st)
                    MBt = sb("MBt", [128, S], BF16, st)
                    junk = sb("junk", [128, D], BF16, st)
                    rts = [sb("rts%d" % i, [128, 512], F32, st) for i in range(2)]
                    pTs1 = [sb("pTa%d" % i, [128, 512], BF16, st) for i in range(5)]
                    pTs2 = [sb("pTb%d" % i, [128, 512], BF16, st) for i in range(3)]
                    yts = [sb("yt%d" % i, [128, 768], BF16, st) for i in range(2)]
                    rinv1 = sb("rinv1", [128, 4], F32, st)
                    rinv2 = sb("rinv2", [128, 4], F32, st)
                    yTs = sb("yTs", [128, 6, 128], BF16, st)

                    wsrc = w_in_d[l].rearrange("(k p) n -> p k n", p=128)
                    for nm in ("qa", "ka", "qb", "kb", "va", "vb", "qc", "qi", "kc", "ki", "vc", "fb", "wi"):
                        DMA("pool", wq[:, :, DST[nm]:DST[nm] + WID[nm]], wsrc[:, :, OFF[nm]:OFF[nm] + WID[nm]], [], [wq.b], wq.b)
                    MS("dve", Va[:, :, :, 64:65], 1.0, [Va.b])
                    MS("dve", Vb[:, :, :, 64:65], 1.0, [Vb.b])
                    MS("dve", Vc[:, :, 64:65], 1.0, vcb)
                    MS("dve", zqb[:, :, 67:70], 1.0, [zqb.b])
                    MS("dve", zkb[:, :, 64:67], 1.0, [zkb.b])
                    MS("dve", cum[1][:, :], 0.0, [cum[1].b])

                    cnt1 = [0, 0]
                    cnt2 = [0, 0]

                    def recip_norm(acc, rinv, ydst):
                        accv = acc[:, 0:260].rearrange("p (h d) -> p h d", h=4)
                        pg.op("dve", lambda e: e.reciprocal(out=rinv[:, :], in_=accv[:, :, 64]), r=[acc.b], w=[rinv.b])
                        return accv

                    def stage1(t):
                        xt = xts[t % 2]
                        cs = css[t % 2]
                        fo = fos[t % 2]
                        QcT = QcTs[t % 2]
                        QiT = QiTs[t % 2]
                        yt = yts[t % 2]
                        DMA("sp", xt[:, :], xin_d[s, t * 128:(t + 1) * 128, :], [], [xt.b], xt.b)
                        DMA("sp", cs[:, :], cs_d[t], [], [cs.b], cs.b)
                        norm_hT(xt, xn, junk, ss, lambda k: (hT[:, k, :], hT.b), A1, B1, PS[0], PS[7])
                        yield
                        cosk = cs[:, 0:32]
                        sink = cs[:, 32:64]
                        cosq = cs[:, 64:96]
                        sinq = cs[:, 96:128]
                        blocks = [(0, 512), (512, 512), (1024, 512), (1536, 512), (2048, 200)]
                        for bi, (c0, cw) in enumerate(blocks):
                            ps = PS[0] if bi % 2 == 0 else PS[7]
                            for k in range(KC):
                                MM(ps[:, 0:cw], hT[:, k, :], wq[:, k, c0:c0 + cw], k == 0, k == KC - 1, [hT.b, wq.b], [ps.b])
                            if bi == 0:
                                TS("dve", zqa[:, :], ps[:, 0:256], 0.125, None, ALU.mult, None, [ps.b], [zqa.b])
                                CP("act", zka[:, :], ps[:, 256:512], [ps.b], [zka.b])
                            elif bi == 1:
                                TS("dve", zqb[:, :, 0:64], ps[:, 0:256].rearrange("p (h d) -> p h d", h=4), 0.125, None,
                                   ALU.mult, None, [ps.b], [zqb.b])
                                CP("act", zkb[:, :, 0:64], ps[:, 256:512].rearrange("p (h d) -> p h d", h=4), [ps.b], [zkb.b])
                            elif bi == 2:
                                CP("act", Va[:, t % 8, :, 0:64], ps[:, 0:256].rearrange("p (h d) -> p h d", h=4), [ps.b], [Va.b])
                                CP("dve", Vb[:, t, :, 0:64], ps[:, 256:512].rearrange("p (h d) -> p h d", h=4), [ps.b], [Vb.b])
                            elif bi == 3:
                                z3 = ps[:, 0:512].rearrange("p (h d) -> p h d", h=8)
                                x1 = z3[:, :, 0:32]
                                x2 = z3[:, :, 32:64]
                                cb = cosq.unsqueeze(1).to_broadcast([128, 8, 32])
                                sbb = sinq.unsqueeze(1).to_broadcast([128, 8, 32])
                                TT("dve", rt1[:, :, :], x1, cb, ALU.mult, [ps.b, cs.b], [rt1.b])
                                TT("dve", rt2[:, :, :], x2, sbb, ALU.mult, [ps.b, cs.b], [rt2.b])
                                TT("pool", zrq[:, :, 0:32], rt1[:, :, :], rt2[:, :, :], ALU.subtract, [rt1.b, rt2.b], [zrq.b])
                                TT("dve", rt1[:, :, :], x1, sbb, ALU.mult, [ps.b, cs.b], [rt1.b])
                                TT("dve", rt2[:, :, :], x2, cb, ALU.mult, [ps.b, cs.b], [rt2.b])
                                TT("pool", zrq[:, :, 32:64], rt1[:, :, :], rt2[:, :, :], ALU.add, [rt1.b, rt2.b], [zrq.b])
                            else:
                                z4 = ps[:, 0:128].rearrange("p (h d) -> p h d", h=2)
                                x1 = z4[:, :, 0:32]
                                x2 = z4[:, :, 32:64]
                                cb = cosk.unsqueeze(1).to_broadcast([128, 2, 32])
                                sbb = sink.unsqueeze(1).to_broadcast([128, 2, 32])
                                TT("dve", rt1[:, 0:2, :], x1, cb, ALU.mult, [ps.b, cs.b], [rt1.b])
                                TT("dve", rt2[:, 0:2, :], x2, sbb, ALU.mult, [ps.b, cs.b], [rt2.b])
                                TT("pool", zrk[:, :, 0:32], rt1[:, 0:2, :], rt2[:, 0:2, :], ALU.subtract, [rt1.b, rt2.b], [zrk.b])
                                TT("dve", rt1[:, 0:2, :], x1, sbb, ALU.mult, [ps.b, cs.b], [rt1.b])
                                TT("dve", rt2[:, 0:2, :], x2, cb, ALU.mult, [ps.b, cs.b], [rt2.b])
                                TT("pool", zrk[:, :, 32:64], rt1[:, 0:2, :], rt2[:, 0:2, :], ALU.add, [rt1.b, rt2.b], [zrk.b])
                                CP("act", Vc[:, t, 0:64], ps[:, 128:192], [ps.b], [vcb[t]])
                                TT("dve", fo[:, 0:4], ps[:, 192:196], b_forgetB[:, l, :], ALU.add, [ps.b, small.b], [fo.b])
                                ACT(fo[:, 16:20], ps[:, 196:200], AF.Abs, [ps.b], [fo.b], scale=0.5)
                                TS("dve", fo[:, 20:24], ps[:, 196:200], 0.0, 2.0, ALU.is_ge, ALU.mult, [ps.b], [fo.b])
                                TS("dve", fo[:, 20:24], fo[:, 20:24], -1.0, None, ALU.add, None, [fo.b], [fo.b])
                                ACT(fo[:, 4:8], fo[:, 0:4], AF.Exp, [fo.b], [fo.b], scale=-1.0)
                                ACT(fo[:, 8:12], fo[:, 4:8], AF.Ln, [fo.b], [fo.b], bias=1.0)
                            yield
                        cprev = cum[(t + 1) % 2]
                        ccur = cum[t % 2]
                        psc = PS[6]
                        MM(psc[:, 0:4], triF, fo[:, 8:12], True, False, [cst.b, fo.b], [psc.b])
                        MM(psc[:, 0:4], sel127, cprev[:, :], False, True, [cst.b, cprev.b], [psc.b])
                        CP("dve", ccur[:, :], psc[:, 0:4], [psc.b], [ccur.b])
                        CP("dve", fob[:, 0:4], ccur[:, :], [ccur.b], [fob.b])
                        TT("dve", fo[:, 24:28], ccur[:, :], fob[:, 0:4], ALU.subtract, [ccur.b, fob.b], [fo.b])
                        CP("dve", fob[:, 4:8], fo[:, 24:28], [fo.b], [fob.b])
                        TT("dve", fo[:, 28:32], fo[:, 24:28], fob[:, 4:8], ALU.subtract, [fo.b, fob.b], [fo.b])
                        CP("dve", fob[:, 8:12], fo[:, 28:32], [fo.b], [fob.b])
                        for i3 in range(3):
                            src3 = fob[:, 4 * i3:4 * i3 + 4].unsqueeze(2)
                            CP("dve", zkb[:, :, 67 + i3:68 + i3], src3, [fob.b], [zkb.b])
                            TS("dve", zqb[:, :, 64 + i3:65 + i3], src3, -1.0, None, ALU.mult, None, [fob.b], [zqb.b])
                        yield
                        pst = PSB[7]
                        pstb = PS[7].b
                        for h in range(4):
                            TR(pst[0:64, h * 128:(h + 1) * 128], zqa[:, h * 64:(h + 1) * 64], identB[:, :], [zqa.b, identB.b], [pstb])
                            TR(pst[0:64, 512 + h * 128:512 + (h + 1) * 128], zka[:, h * 64:(h + 1) * 64], identB[:, :], [zka.b, identB.b], [pstb])
                        CP("dve", QaT[:, :, :], pst[0:64, 0:512].rearrange("p (h q) -> p h q", h=4), [pstb], [QaT.b])
                        CP("act", KaT[:, :, (t % 8) * 128:(t % 8 + 1) * 128], pst[0:64, 512:1024].rearrange("p (h q) -> p h q", h=4), [pstb], [KaT.b])
                        yield
                        for h in range(4):
                            TR(pst[0:70, h * 128:(h + 1) * 128], zqb[:, h, :], identB[:, :], [zqb.b, identB.b], [pstb])
                            TR(pst[0:70, 512 + h * 128:512 + (h + 1) * 128], zkb[:, h, :], identB[:, :], [zkb.b, identB.b], [pstb])
                        CP("dve", QbT[:, :, :], pst[0:70, 0:512].rearrange("p (h q) -> p h q", h=4), [pstb], [QbT.b])
                        CP("act", KbT[:, :, t * 128:(t + 1) * 128], pst[0:70, 512:1024].rearrange("p (h q) -> p h q", h=4), [pstb], [KbT.b])
                        yield
                        for h in range(4):
                            TR(pst[0:64, h * 128:(h + 1) * 128], zrq[:, h, :], identB[:, :], [zrq.b, identB.b], [pstb])
                            TR(pst[0:64, 512 + h * 128:512 + (h + 1) * 128], zrq[:, 4 + h, :], identB[:, :], [zrq.b, identB.b], [pstb])
                        CP("dve", QcT[:, :, :], pst[0:64, 0:512].rearrange("p (h q) -> p h q", h=4), [pstb], [QcT.b])
                        CP("act", QiT[:, :, :], pst[0:64, 512:1024].rearrange("p (h q) -> p h q", h=4), [pstb], [QiT.b])
                        TR(pst[0:64, 0:128], zrk[:, 0, :], identB[:, :], [zrk.b, identB.b], [pstb])
                        TR(pst[0:64, 128:256], zrk[:, 1, :], identB[:, :], [zrk.b, identB.b], [pstb])
                        CP("dve", KcT[:, t * 128:(t + 1) * 128], pst[0:64, 0:128], [pstb], [kcb[t]])
                        CP("act", KiT[:, t * 128:(t + 1) * 128], pst[0:64, 128:256], [pstb], [kib[t]])
                        yield
                        acc = PS[6]

                        def nS():
                            cnt1[0] += 1
                            return PS[4 + cnt1[0] % 2]

                        def nP():
                            cnt1[1] += 1
                            return pTs1[cnt1[1] % 5]
                        if 'a' not in skip:
                            mlist = [m for m in range(5) if t - 4 + m >= 0]
                            pa = {}
                            for m in mlist:
                                sl = (t - 4 + m) % 8
                                sps = nS()
                                for h in range(4):
                                    MM(sps[:, h * 128:(h + 1) * 128], KaT[:, h, sl * 128:(sl + 1) * 128],
                                       QaT[:, h, :], h == 0, False, [KaT.b, QaT.b], [sps.b])
                                MM(sps[:, :], identB[:, :], biasAll[:, l, m * 512:(m + 1) * 512], False, True, [identB.b, biasAll.b], [sps.b])
                                p = nP()
                                ACT(p[:, :], sps[:, :], AF.Exp, [sps.b], [p.b])
                                pa[m] = p
                                yield
                            first = True
                            for h in range(4):
                                for m in mlist:
                                    sl = (t - 4 + m) % 8
                                    MM(acc[:, h * 65:(h + 1) * 65], pa[m][:, h * 128:(h + 1) * 128], Va[:, sl, h, :], first, False,
                                       [pa[m].b, Va.b], [acc.b])
                                    first = False
                            accv = recip_norm(acc, rinv1, yt)
                            TT("dve", yt[:, 0:256].rearrange("p (h d) -> p h d", h=4), accv[:, :, 0:64],
                               rinv1[:, :].unsqueeze(2).to_broadcast([128, 4, 64]), ALU.mult, [acc.b, rinv1.b], [yt.b])
                            yield
                        if 'b' not in skip:
                            def qk_b(j):
                                sps = nS()
                                for h in range(4):
                                    MM(sps[:, h * 128:(h + 1) * 128], KbT[:, h, j * 128:(j + 1) * 128], QbT[:, h, :], h == 0, False,
                                       [KbT.b, QbT.b], [sps.b])
                                if j == t:
                                    MM(sps[:, :], identB[:, :], tribB[:, :], False, True, [identB.b, tribB.b], [sps.b])
                                p = nP()
                                ACT(p[:, :], sps[:, :], AF.Exp, [sps.b], [p.b])
                                return p

                            def pv_b(j, p, first):
                                for h in range(4):
                                    MM(acc[:, h * 65:(h + 1) * 65], p[:, h * 128:(h + 1) * 128], Vb[:, j, h, :], first and h == 0, False,
                                       [p.b, Vb.b], [acc.b])
                            pprev = qk_b(0)
                            for j in range(1, t + 1):
                                pcur = qk_b(j)
                                pv_b(j - 1, pprev, j == 1)
                                pprev = pcur
                                yield
                            pv_b(t, pprev, t == 0)
                            yield
                            accv = recip_norm(acc, rinv1, yt)
                            TT("dve", yt[:, 256:512].rearrange("p (h d) -> p h d", h=4), accv[:, :, 0:64],
                               rinv1[:, :].unsqueeze(2).to_broadcast([128, 4, 64]), ALU.mult, [acc.b, rinv1.b], [yt.b])
                            yield

                    def stage2(t):
                        fo = fos[t % 2]
                        QcT = QcTs[t % 2]
                        QiT = QiTs[t % 2]
                        yt = yts[t % 2]
                        acc = PS[1]

                        def nS():
                            cnt2[0] += 1
                            return PS[2 + cnt2[0] % 2]

                        def nP():
                            cnt2[1] += 1
                            return pTs2[cnt2[1] % 3]
                        if 'c' not in skip:
                            N = 128 * (t + 1)
                            nkb = (N + 511) // 512
                            for kb in range(nkb):
                                cw = min(512, N - kb * 512)
                                kbufs = kib[kb * 4:min(kb * 4 + 4, t + 1)]
                                for h in range(4):
                                    sps = nS()
                                    MM(sps[:, 0:cw], QiT[:, h, :], KiT[:, kb * 512:kb * 512 + cw], True, True, [QiT.b] + kbufs, [sps.b])
                                    rt = rts[h % 2]
                                    ACT(rt[:, 0:cw], sps[:, 0:cw], AF.Relu, [sps.b, fo.b], [rt.b], scale=fo[:, 16 + h:17 + h])
                                    sc = score[:, kb * 512:kb * 512 + cw]
                                    if h == 0:
                                        TS("dve", sc, rt[:, 0:cw], fo[:, 20:21], None, ALU.mult, None, [rt.b, fo.b], [score.b])
                                    else:
                                        STT("dve", sc, rt[:, 0:cw], fo[:, 20 + h:21 + h], sc, ALU.mult, ALU.add, [rt.b, fo.b, score.b], [score.b])
                                    yield
                            if N <= TOPK or 'n' in skip:
                                MS("dve", bis[:, 0:1], -1e29, [bis.b])
                                MS("dve", score[0:64, t * 128 + 64:(t + 1) * 128], -1e30, [score.b])
                            else:
                                pg.op("dve", lambda e, N=N: e.tensor_reduce(out=bis[:, 1:2], in_=score[:, 0:N], axis=AX.X, op=ALU.max), r=[score.b], w=[bis.b])
                                pg.op("dve", lambda e, N=N: e.tensor_reduce(out=bis[:, 2:3], in_=score[:, 0:N], axis=AX.X, op=ALU.min), r=[score.b], w=[bis.b])
                                MS("dve", score[0:64, t * 128 + 64:(t + 1) * 128], -1e30, [score.b])
                                TT("dve", bis[:, 3:4], bis[:, 1:2], bis[:, 2:3], ALU.subtract, [bis.b], [bis.b])
                                TS("dve", bis[:, 8:8 + NIT], pow2, bis[:, 3:4], None, ALU.mult, None, [cst.b, bis.b], [bis.b])
                                TT("dve", bis[:, 4:5], bis[:, 2:3], bis[:, 8:9], ALU.add, [bis.b], [bis.b])
                                yield
                                for it in range(NIT):
                                    TS("dve", MBt[:, 0:N], score[:, 0:N], bis[:, 4:5], None, ALU.is_ge, ALU.add, [score.b, bis.b],
                                       [MBt.b, bis.b], accum_out=bis[:, 5:6])
                                    if it < NIT - 1:
                                        TS("dve", bis[:, 6:7], bis[:, 5:6], TOPK - 0.5, 0.5, ALU.is_ge, ALU.subtract, [bis.b], [bis.b])
                                        STT("dve", bis[:, 4:5], bis[:, 6:7], bis[:, 8 + it:9 + it], bis[:, 4:5], ALU.mult, ALU.add,
                                            [bis.b], [bis.b])
                                    else:
                                        TS("dve", bis[:, 6:7], bis[:, 5:6], TOPK - 0.5, 1.0, ALU.is_ge, ALU.subtract, [bis.b], [bis.b])
                                        STT("dve", bis[:, 0:1], bis[:, 6:7], bis[:, 8 + it:9 + it], bis[:, 4:5], ALU.mult, ALU.add,
                                            [bis.b], [bis.b])
                                    yield
                            TS("dve", MBt[:, 0:N], score[:, 0:N], bis[:, 0:1], NEGM, ALU.is_lt, ALU.mult, [score.b, bis.b], [MBt.b])
                            yield
                            def qk_c(j):
                                sps = nS()
                                MM(sps[:, :], KcT[:, j * 128:(j + 1) * 128], QcT[:, :, :].rearrange("p h q -> p (h q)"), True, False,
                                   [kcb[j], QcT.b], [sps.b])
                                MM(sps[:, :], MBt[:, j * 128:(j + 1) * 128], i4B[:, :], False, True, [MBt.b, i4B.b], [sps.b])
                                p = nP()
                                ACT(p[:, :], sps[:, :], AF.Exp, [sps.b], [p.b])
                                return p

                            def pv_c(j, p, first):
                                for h in range(4):
                                    MM(acc[:, h * 65:(h + 1) * 65], p[:, h * 128:(h + 1) * 128], Vc[:, j, :], first and h == 0, False,
                                       [p.b, vcb[j]], [acc.b])
                            pprev = qk_c(0)
                            for j in range(1, t + 1):
                                pcur = qk_c(j)
                                pv_c(j - 1, pprev, j == 1)
                                pprev = pcur
                                yield
                            pv_c(t, pprev, t == 0)
                            yield
                            accv = recip_norm(acc, rinv2, yt)
                            TT("dve", yt[:, 512:768].rearrange("p (h d) -> p h d", h=4), accv[:, :, 0:64],
                               rinv2[:, :].unsqueeze(2).to_broadcast([128, 4, 64]), ALU.mult, [acc.b, rinv2.b], [yt.b])
                        pst2 = PSB[3]
                        pst2b = PS[3].b
                        for c6 in range(6):
                            TR(pst2[:, c6 * 128:(c6 + 1) * 128], yt[:, c6 * 128:(c6 + 1) * 128], identB[:, :], [yt.b, identB.b], [pst2b])
                        CP("act", yTs[:, :, :], pst2[:, 0:768].rearrange("p (c q) -> p c q", c=6), [pst2b], [yTs.b])
                        DMA("sp", yT_d[s, :, :, t * 128:(t + 1) * 128], yTs[:, :, :], [yTs.b], [], yTs.b)
                        yield

                    def interleave(g1, g2):
                        d1 = g1 is None
                        d2 = g2 is None
                        while not (d1 and d2):
                            if not d1:
                                try:
                                    next(g1)
                                except StopIteration:
                                    d1 = True
                            if not d2:
                                try:
                                    next(g2)
                                except StopIteration:
                                    d2 = True

                    def inter_prop(g1, n1, g2, n2):
                        c1 = c2 = 0
                        d1 = g1 is None
                        d2 = g2 is None
                        while not (d1 and d2):
                            pick1 = (not d1) and (d2 or (c1 + 1) * n2 <= (c2 + 1) * n1)
                            if pick1:
                                try:
                                    next(g1)
                                    c1 += 1
                                except StopIteration:
                                    d1 = True
                            else:
                                try:
                                    next(g2)
                                    c2 += 1
                                except StopIteration:
                                    d2 = True

                    for t in range(NT):
                        n1 = 17 + t
                        n2 = 4 * ((t + 3) // 4) + NIT + 4 + t
                        inter_prop(stage1(t), n1, stage2(t - 1) if t >= 1 else None, n2)
                    interleave(None, stage2(NT - 1))
                    pg.barrier()
                    pg.emit()
                if stop_after == "AB":
                    continue
                GC = 512 if S % 512 == 0 else 256
                with ExitStack() as st:
                    wg = sb("wg", [128, KC, 3072], BF16, st)
                    wb = sb("wb", [128, 6, D], BF16, st)
                    wo = sb("wo", [128, KC, D], BF16, st)
                    NQ = GC // 128
                    xt4s = [[sb("xc%d_%d" % (j, i), [128, D], F32, st) for i in range(NQ)] for j in range(2)]
                    xn = sb("xn", [128, D], F32, st)
                    junk = sb("junk", [128, D], BF16, st)
                    hT4s = [sb("hT4_%d" % j, [128, KC, GC], BF16, st) for j in range(2)]
                    yT4s = [sb("yT4_%d" % j, [128, 6, GC], BF16, st) for j in range(2)]
                    gts = [sb("gt%d" % i, [128, GC], F32, st) for i in range(3)]
                    tm = [sb("tm%d" % i, [128, GC], F32, st) for i in range(2)]
                    mT = sb("mT", [128, KC, GC], BF16, st)
                    ots = [sb("ot%d" % i, [128, D], F32, st) for i in range(2)]
                    ss = sb("ss", [128, 8], F32, st)
                    ss2 = sb("ss2", [128, 8], F32, st)
                    junk2 = sb("junk2", [128, D], BF16, st)
                    wsrc = w_in_d[l].rearrange("(k p) n -> p k n", p=128)
                    for br in range(3):
                        DMA("pool", wg[:, :, br * 1024:(br + 1) * 1024], wsrc[:, :, OFF["zg"] + br * 1024:OFF["zg"] + (br + 1) * 1024],
                            [], [wg.b], wg.b)
                    DMA("pool", wb[:, :, :], w_branch_d[l].rearrange("b (k p) n -> p (b k) n", p=128), [], [wb.b], wb.b)
                    DMA("pool", wo[:, :, :], w_out_d[l].rearrange("(k p) n -> p k n", p=128), [], [wo.b], wo.b)
                    G1 = sb("G1", [128, D], F32, st)
                    DMA("sp", G1[:, :], G_d[l, s, 0], [], [G1.b], G1.b)
                    oic = [0]

                    def c_norm(g):
                        t0 = g * GC
                        hT4 = hT4s[g % 2]
                        yT4 = yT4s[g % 2]
                        DMA("sp", yT4[:, :, :], yT_d[s, :, :, t0:t0 + GC], [], [yT4.b], yT4.b)
                        for q in range(NQ):
                            xt = xt4s[g % 2][q]
                            DMA("sp", xt[:, :], xin_d[s, t0 + q * 128:t0 + (q + 1) * 128, :], [], [xt.b], xt.b)
                            norm_hT(xt, xn, junk, ss, (lambda q: lambda k: (hT4[:, k, q * 128:(q + 1) * 128], hT4.b))(q), A1, B1, PS[0], PS[1])
                            yield

                    def c_main(g):
                        t0 = g * GC
                        hT4 = hT4s[g % 2]
                        yT4 = yT4s[g % 2]
                        for fc in range(KC):
                            pps = []
                            for br in range(3):
                                ps = PS[2 + br]
                                for k in range(KC):
                                    MM(ps[:, 0:GC], wg[:, k, br * 1024 + fc * 128:br * 1024 + (fc + 1) * 128], hT4[:, k, :], k == 0, k == KC - 1,
                                       [wg.b, hT4.b], [ps.b])
                                ACT(gts[br][:, :], ps[:, 0:GC], AF.Sigmoid, [ps.b, small.b], [gts[br].b], bias=b_gateT[:, l, br, fc:fc + 1])
                                pp = PS[5 + br]
                                for kk in range(2):
                                    MM(pp[:, 0:GC], wb[:, br * 2 + kk, fc * 128:(fc + 1) * 128], yT4[:, br * 2 + kk, :], kk == 0, kk == 1,
                                       [wb.b, yT4.b], [pp.b])
                                pps.append(pp)
                            TT("dve", tm[0][:, :], gts[0][:, :], pps[0][:, 0:GC], ALU.mult, [gts[0].b, pps[0].b], [tm[0].b])
                            TT("dve", tm[1][:, :], gts[1][:, :], pps[1][:, 0:GC], ALU.mult, [gts[1].b, pps[1].b], [tm[1].b])
                            TT("pool", tm[0][:, :], tm[0][:, :], tm[1][:, :], ALU.add, [tm[0].b, tm[1].b], [tm[0].b])
                            TT("dve", tm[1][:, :], gts[2][:, :], pps[2][:, 0:GC], ALU.mult, [gts[2].b, pps[2].b], [tm[1].b])
                            TT("pool", mT[:, fc, :], tm[0][:, :], tm[1][:, :], ALU.add, [tm[0].b, tm[1].b], [mT.b])
                            yield
                        for q in range(NQ):
                            for half in range(2):
                                ps = PS[2 + half]
                                for k in range(KC):
                                    MM(ps[:, :], mT[:, k, q * 128:(q + 1) * 128], wo[:, k, half * 512:(half + 1) * 512], k == 0, k == KC - 1,
                                       [mT.b, wo.b], [ps.b])
                            ot = ots[oic[0] % 2]
                            oic[0] += 1
                            postnorm_residual(PS[2], PS[3], xt4s[g % 2][q], G1, ss2, ot, junk2)
                            DMA("sp", x1_d[s, t0 + q * 128:t0 + (q + 1) * 128, :], ot[:, :], [ot.b], [], ot.b)
                            yield

                    NGC = S // GC
                    interleave(c_norm(0), None)
                    for g in range(NGC):
                        interleave(c_main(g), c_norm(g + 1) if g + 1 < NGC else None)
                    pg.barrier()
                    pg.emit()
                if stop_after == "C":
                    continue
                with ExitStack() as st:
                    wu = sb("wu", [128, KC, 2 * DFF], BF16, st)
                    wd = sb("wd", [128, FC, D], BF16, st)
                    xts = [sb("xd%d" % i, [128, D], F32, st) for i in range(1)]
                    xrs = [sb("xr%d" % i, [128, D], F32, st) for i in range(1)]
                    xn = sb("xn", [128, D], F32, st)
                    junk = sb("junk", [128, D], BF16, st)
                    junk2 = sb("junk2", [128, D], BF16, st)
                    hT2s = [sb("hT2_%d" % j, [128, KC, GD], BF16, st) for j in range(2)]
                    actT = sb("actT", [128, FC, GD], BF16, st)
                    araw = [sb("araw%d" % i, [128, GD + 2], F32, st) for i in range(2)]
                    cv = [sb("cv%d" % i, [128, GD], F32, st) for i in range(2)]
                    ge = [sb("ge%d" % i, [128, GD], F32, st) for i in range(2)]
                    halo = sb("halo", [128, FC, 2], F32, st)
                    gsb = [sb("gs%d" % i, [128, GD], F32, st) for i in range(2)]
                    ots = [sb("ot%d" % i, [128, D], F32, st) for i in range(2)]
                    ss = sb("ss", [128, 8], F32, st)
                    ss2 = sb("ss2", [128, 8], F32, st)
                    usrc = w_up_d[l].rearrange("(k p) n -> p k n", p=128)
                    for c0 in range(0, 2 * DFF, 1408):
                        DMA("pool", wu[:, :, c0:c0 + 1408], usrc[:, :, c0:c0 + 1408], [], [wu.b], wu.b)
                    dsrc = w_down_d[l].rearrange("(k p) n -> p k n", p=128)
                    for k0 in range(0, FC, 11):
                        DMA("pool", wd[:, k0:k0 + 11, :], dsrc[:, k0:k0 + 11, :], [], [wd.b], wd.b)
                    MS("dve", halo[:, :, :], 0.0, [halo.b])
                    G2 = sb("G2", [128, D], F32, st)
                    DMA("sp", G2[:, :], G_d[l, s, 1], [], [G2.b], G2.b)
                    oid = [0]

                    def d_norm(g):
                        t0 = g * GD
                        hT2 = hT2s[g % 2]
                        for q in range(GD // 128):
                            xt = xts[0]
                            DMA("sp", xt[:, :], x1_d[s, t0 + q * 128:t0 + (q + 1) * 128, :], [], [xt.b], xt.b)
                            norm_hT(xt, xn, junk, ss, (lambda q: lambda k: (hT2[:, k, q * 128:(q + 1) * 128], hT2.b))(q), A2, B2, PS[0], PS[1])
                            yield

                    def d_main(g):
                        t0 = g * GD
                        hT2 = hT2s[g % 2]
                        for fc in range(FC):
                            pa_ = PS[2 + (fc % 2) * 2]
                            pgt = PS[3 + (fc % 2) * 2]
                            for k in range(KC):
                                MM(pa_[:, 0:GD], wu[:, k, fc * 128:(fc + 1) * 128], hT2[:, k, :], k == 0, k == KC - 1, [wu.b, hT2.b], [pa_.b])
                            for k in range(KC):
                                MM(pgt[:, 0:GD], wu[:, k, DFF + fc * 128:DFF + (fc + 1) * 128], hT2[:, k, :], k == 0, k == KC - 1,
                                   [wu.b, hT2.b], [pgt.b])
                            ar = araw[fc % 2]
                            c_ = cv[fc % 2]
                            g_ = ge[fc % 2]
                            gs_ = gsb[fc % 2]
                            CP("dve", gs_[:, :], pgt[:, 0:GD], [pgt.b], [gs_.b])
                            CP("pool", ar[:, 0:2], halo[:, fc, :], [halo.b], [ar.b])
                            CP("act", ar[:, 2:GD + 2], pa_[:, 0:GD], [pa_.b], [ar.b])
                            CP("pool", halo[:, fc, :], ar[:, GD:GD + 2], [ar.b], [halo.b])
                            ACT(c_[:, :], ar[:, 2:GD + 2], AF.Identity, [ar.b, small.b], [c_.b], bias=conv_bT[:, l, fc:fc + 1],
                                scale=conv_wT[:, l, 2, fc:fc + 1])
                            STT("dve", c_[:, :], ar[:, 1:GD + 1], conv_wT[:, l, 1, fc:fc + 1], c_[:, :], ALU.mult, ALU.add, [ar.b, small.b, c_.b], [c_.b])
                            STT("dve", c_[:, :], ar[:, 0:GD], conv_wT[:, l, 0, fc:fc + 1], c_[:, :], ALU.mult, ALU.add, [ar.b, small.b, c_.b], [c_.b])
                            ACT(g_[:, :], c_[:, :], AF.Gelu_apprx_tanh, [c_.b], [g_.b])
                            TT("dve", actT[:, fc, :], g_[:, :], gs_[:, :], ALU.mult, [g_.b, gs_.b], [actT.b])
                            yield
                        for q in range(GD // 128):
                            xr = xrs[0]
                            DMA("sp", xr[:, :], x1_d[s, t0 + q * 128:t0 + (q + 1) * 128, :], [], [xr.b], xr.b)
                            for half in range(2):
                                ps = PS[6 + half]
                                for k in range(FC):
                                    MM(ps[:, :], actT[:, k, q * 128:(q + 1) * 128], wd[:, k, half * 512:(half + 1) * 512], k == 0, k == FC - 1,
                                       [actT.b, wd.b], [ps.b])
                            ot = ots[oid[0] % 2]
                            oid[0] += 1
                            postnorm_residual(PS[6], PS[7], xr, G2, ss2, ot, junk2)
                            DMA("sp", xout_d[s, t0 + q * 128:t0 + (q + 1) * 128, :], ot[:, :], [ot.b], [], ot.b)
                            yield

                    NGD = S // GD
                    interleave(d_norm(0), None)
                    for g in range(NGD):
                        interleave(d_main(g), d_norm(g + 1) if g + 1 < NGD else None)
                    pg.barrier()
                    pg.emit()
    return nc


def _consts(S):
    NT = S // 128
    cst = np.zeros((128, NCST), np.float32)
    k = np.arange(128)[:, None]
    m = np.arange(128)[None, :]
    cst[:, 0:128] = (k == m)
    cst[:, 128:256] = (k <= m)
    cst[:, 256:384] = (k == 127)
    cst[:, 384:896] = np.tile((k == m).astype(np.float32), (1, 4))
    cst[:, 896:1408] = np.tile(np.where(k > m, NEGM, 0.0).astype(np.float32), (1, 4))
    cst[:, 1408:1408 + NIT] = (2.0 ** -(np.arange(NIT) + 1.0))[None, :]
    pos = np.arange(S, dtype=np.float32)
    inv_freq = (np.float32(10000.0) ** (-np.arange(0, 64, 2, dtype=np.float32) / np.float32(64))).astype(np.float32)
    ang = (pos[:, None] * inv_freq[None, :]).astype(np.float32)
    cos = np.cos(ang).astype(np.float32)
    sin = np.sin(ang).astype(np.float32)
    cs = np.concatenate([cos, sin, cos * np.float32(0.125), sin * np.float32(0.125)], axis=1).reshape(NT, 128, 128)
    ki = np.arange(128)[:, None, None]
    mm = np.arange(5)[None, :, None]
    qi = np.arange(128)[None, None, :]
    rel = 512 - 128 * mm + qi - ki
    qm = qi % 64
    valid = (rel >= qm - 63) & (rel <= qm + 512)
    relidx = np.clip(rel, -128, 128) + 128
    maskA = np.where(valid, 0.0, NEGM).astype(np.float32)
    maskA = np.broadcast_to(maskA[:, :, None, :], (128, 5, 4, 128)).reshape(128, 5 * 512)
    return cst, cs.astype(np.float32), relidx, np.ascontiguousarray(maskA)


def _prep(inputs, S, NSEQ, ncores):
    f = lambda a: np.ascontiguousarray(np.asarray(a, dtype=np.float32))
    cst, cs, relidx, maskA = _consts(S)
    x = f(inputs["x"])
    c = f(inputs["c"])
    b_ada = f(inputs["b_ada"])
    norm_g = f(inputs["norm_g"])
    rel_table = f(inputs["rel_table"])
    shared = {
        "w_ada": f(inputs["w_ada"]),
        "b_adaT": np.ascontiguousarray(b_ada.reshape(2, 48, 128).transpose(2, 0, 1)),
        "b_adaB": np.ascontiguousarray(np.broadcast_to(
            b_ada.reshape(2, 6, D)[:, [2, 5], :][None], (128, 2, 2, D))),
        "normgT": np.ascontiguousarray(norm_g.reshape(2, 4, KC, 128).transpose(3, 0, 1, 2)),
        "normgB": np.ascontiguousarray(np.broadcast_to(norm_g[:, [1, 3], :][None], (128, 2, 2, D))),
        "w_in": f(inputs["w_in"]),
        "b_gateT": np.ascontiguousarray(f(inputs["b_gate"]).reshape(2, 3, KC, 128).transpose(3, 0, 1, 2)),
        "biasT": np.ascontiguousarray(rel_table[:, :, relidx].transpose(0, 2, 3, 1, 4).reshape(2, 128, 5 * 512)),
        "maskA": maskA,
        "b_forgetB": np.ascontiguousarray(np.broadcast_to(f(inputs["b_forget"])[None], (128, 2, 4))),
        "w_branch": f(inputs["w_branch"]),
        "w_out": f(inputs["w_out"]),
        "w_up": f(inputs["w_up"]),
        "conv_wT": np.ascontiguousarray(f(inputs["conv_w"]).reshape(2, 3, FC, 128).transpose(3, 0, 1, 2)),
        "conv_bT": np.ascontiguousarray(f(inputs["conv_b"]).reshape(2, FC, 128).transpose(2, 0, 1)),
        "w_down": f(inputs["w_down"]),
        "cs": cs,
        "cst": cst,
    }
    maps = []
    for i in range(ncores):
        d = dict(shared)
        d["x"] = np.ascontiguousarray(x[i * NSEQ:(i + 1) * NSEQ])
        cc = c[i * NSEQ:(i + 1) * NSEQ]
        d["cT"] = np.ascontiguousarray(cc.reshape(NSEQ, KC, 128).transpose(2, 1, 0))
        maps.append(d)
    return maps


def run(inputs, cfg, ncores=8):
    nc = build(cfg)
    maps = _prep(inputs, cfg["S"], cfg["NSEQ"], ncores)
    res = run_bass_kernel_spmd(nc, maps, core_ids=list(range(ncores)))
    return res


def kernel(**inputs):
    x = np.asarray(inputs["x"])
    B, S, _ = x.shape
    ncores = 8
    NSEQ = B // ncores
    cfg = dict(S=S, NSEQ=NSEQ, NL=2)
    res = run(inputs, cfg, ncores)
    out = np.concatenate([np.asarray(r["out"]) for r in res.results], axis=0)
    return out.astype(np.float32)
```
